# Optimizing a Trainium2 kernel written in Bass

```python
import math
import jax, jax.numpy as jnp
from jax import lax
import numpy as np

D_MODEL = 1024
BATCH = 16
SEQ = 2048
DEPTH = 1

CHUNK = 64
EPS = 1e-6

MLA_HEADS = 8
MLA_Q_RANK = 384
MLA_KV_RANK = 256
MLA_NOPE = 64
MLA_ROPE = 32
MLA_V = 64
MLA_QK = MLA_NOPE + MLA_ROPE
ROPE_THETA = 10000.0
Q_BLOCK = 128

RWKV_HEADS = 8
RWKV_HEAD = 64
RWKV_DIM = RWKV_HEADS * RWKV_HEAD
W_LORA = 64
A_LORA = 64
G_LORA = 128
LN_X_EPS = 64e-5

MIX_WIDTH = MLA_HEADS * MLA_V + RWKV_DIM
MLA_COLS = MLA_Q_RANK + MLA_KV_RANK + MLA_ROPE
RWKV_COLS = 3 * RWKV_DIM + W_LORA + A_LORA + G_LORA
IN_COLS = MLA_COLS + RWKV_COLS
MLA_SPLITS = (MLA_Q_RANK, MLA_Q_RANK + MLA_KV_RANK)
RWKV_SPLITS = (RWKV_DIM, 2 * RWKV_DIM, 3 * RWKV_DIM, 3 * RWKV_DIM + W_LORA, 3 * RWKV_DIM + W_LORA + A_LORA)

PEER_HEADS = 8
N_KEYS = 128
N_EXPERTS = N_KEYS * N_KEYS
PEER_DQ = 256
PEER_DHALF = PEER_DQ // 2
PEER_TOPK = 16
TOK_BLOCK = 128

kernel_name = "hybrid_mla_rwkv7_peer_block"


def rms_norm(x, g, eps=EPS):
    xf = x.astype(jnp.float32)
    y = xf * lax.rsqrt(jnp.mean(xf * xf, axis=-1, keepdims=True) + eps)
    return (y * g.astype(jnp.float32)).astype(x.dtype)


def apply_rope(t, positions):
    half = MLA_ROPE // 2
    inv = ROPE_THETA ** (-jnp.arange(half, dtype=jnp.float32) / half)
    ang = positions.astype(jnp.float32)[..., None] * inv
    cos = jnp.cos(ang)[:, :, None, :]
    sin = jnp.sin(ang)[:, :, None, :]
    tf = t.astype(jnp.float32)
    t1, t2 = tf[..., :half], tf[..., half:]
    out = jnp.concatenate([t1 * cos - t2 * sin, t1 * sin + t2 * cos], axis=-1)
    return out.astype(t.dtype)


def mla_group(p_mla, positions, g_cq, g_ckv, w_uq, w_uk, w_uv, g_qnorm, g_knorm, g_attn_out):
    B, S, _ = p_mla.shape
    c_q, c_kv, k_rope = jnp.split(p_mla, MLA_SPLITS, axis=-1)
    q = (rms_norm(c_q, g_cq) @ w_uq).reshape(B, S, MLA_HEADS, MLA_QK)
    c_kv = rms_norm(c_kv, g_ckv)
    k_nope = (c_kv @ w_uk).reshape(B, S, MLA_HEADS, MLA_NOPE)
    v = (c_kv @ w_uv).reshape(B, S, MLA_HEADS, MLA_V)
    k = jnp.concatenate([k_nope, jnp.broadcast_to(k_rope[:, :, None, :], (B, S, MLA_HEADS, MLA_ROPE))], axis=-1)
    q = rms_norm(q, g_qnorm)
    k = rms_norm(k, g_knorm)
    q = jnp.concatenate([q[..., :MLA_NOPE], apply_rope(q[..., MLA_NOPE:], positions)], axis=-1)
    k = jnp.concatenate([k[..., :MLA_NOPE], apply_rope(k[..., MLA_NOPE:], positions)], axis=-1)
    scale = MLA_QK ** -0.5
    n_blk = S // Q_BLOCK
    q_blocks = q.reshape(B, n_blk, Q_BLOCK, MLA_HEADS, MLA_QK).transpose(1, 0, 2, 3, 4)
    key_chunk = jnp.arange(S) // CHUNK

    def attend(args):
        qb, bi = args
        q_chunk = (bi * Q_BLOCK + jnp.arange(Q_BLOCK)) // CHUNK
        s = jnp.einsum('bqhd,bkhd->bhqk', qb, k).astype(jnp.float32) * scale
        mask = key_chunk[None, :] <= q_chunk[:, None]
        s = jnp.where(mask[None, None], s, -jnp.inf)
        pr = jax.nn.softmax(s, axis=-1).astype(v.dtype)
        return jnp.einsum('bhqk,bkhd->bqhd', pr, v)

    o = lax.map(attend, (q_blocks, jnp.arange(n_blk)))
    o = o.transpose(1, 0, 2, 3, 4).reshape(B, S, MLA_HEADS, MLA_V)
    o = rms_norm(o, g_attn_out.reshape(MLA_HEADS, MLA_V))
    return o.reshape(B, S, MLA_HEADS * MLA_V)


def rwkv7_step(state, inp):
    r_t, w_t, k_t, v_t, kk_t, a_t = inp
    sk = jnp.einsum('bhij,bhj->bhi', state, kk_t)
    state = (state * w_t[:, :, None, :]
             - sk[..., None] * (kk_t * a_t)[:, :, None, :]
             + v_t[..., None] * k_t[:, :, None, :])
    y = jnp.einsum('bhij,bhj->bhi', state, r_t)
    return state, y


def rwkv7_group(p_rwkv, rwkv_mu, w0, w2, a0, a2, g2, k_k, k_a, r_k, ln_x_w, ln_x_b):
    B, S, _ = p_rwkv.shape
    prev = jnp.pad(p_rwkv, ((0, 0), (1, 0), (0, 0)))[:, :S]
    p = p_rwkv + (prev - p_rwkv) * rwkv_mu
    r, k, v, wl, al, gl = jnp.split(p, RWKV_SPLITS, axis=-1)
    w = -jax.nn.softplus(-(w0 + jnp.tanh(wl) @ w2)) - 0.5
    decay = jnp.exp(-jnp.exp(w.astype(jnp.float32)))
    a = jax.nn.sigmoid(a0 + al @ a2)
    g = jax.nn.sigmoid(gl) @ g2
    heads = lambda t: t.astype(jnp.float32).reshape(B, S, RWKV_HEADS, RWKV_HEAD)
    kk = heads(k * k_k)
    kk = kk / jnp.maximum(jnp.sqrt(jnp.sum(kk * kk, axis=-1, keepdims=True)), 1e-12)
    k = k * (1.0 + (a - 1.0) * k_a)
    r_h, k_h, v_h, a_h, w_h = heads(r), heads(k), heads(v), heads(a), heads(decay)
    xs = tuple(jnp.moveaxis(t, 1, 0) for t in (r_h, w_h, k_h, v_h, kk, a_h))
    state0 = jnp.zeros((B, RWKV_HEADS, RWKV_HEAD, RWKV_HEAD), jnp.float32)
    _, y = lax.scan(rwkv7_step, state0, xs)
    y = jnp.moveaxis(y, 0, 1)
    mu = jnp.mean(y, axis=-1, keepdims=True)
    var = jnp.mean(jnp.square(y - mu), axis=-1, keepdims=True)
    y = ((y - mu) * lax.rsqrt(var + LN_X_EPS) * ln_x_w.astype(jnp.float32).reshape(RWKV_HEADS, RWKV_HEAD)
         + ln_x_b.astype(jnp.float32).reshape(RWKV_HEADS, RWKV_HEAD))
    bonus = jnp.sum(r_h * k_h * r_k.astype(jnp.float32), axis=-1, keepdims=True) * v_h
    y = (y + bonus).reshape(B, S, RWKV_DIM).astype(p_rwkv.dtype)
    return y * g


def peer_ffn(h, w_pq, sub_keys, expert_u, expert_v):
    B, S, D = h.shape
    hb_all = h.reshape(B * S // TOK_BLOCK, TOK_BLOCK, D)

    def block(hb):
        q = (hb @ w_pq).reshape(TOK_BLOCK, PEER_HEADS, 2, PEER_DHALF)
        s = jnp.einsum('thpd,hpnd->thpn', q, sub_keys).astype(jnp.float32)
        s1, i1 = lax.top_k(s[:, :, 0], PEER_TOPK)
        s2, i2 = lax.top_k(s[:, :, 1], PEER_TOPK)
        cand_s = (s1[..., :, None] + s2[..., None, :]).reshape(TOK_BLOCK, PEER_HEADS, PEER_TOPK * PEER_TOPK)
        cand_i = (i1[..., :, None] * N_KEYS + i2[..., None, :]).reshape(TOK_BLOCK, PEER_HEADS, PEER_TOPK * PEER_TOPK)
        top_s, pos = lax.top_k(cand_s, PEER_TOPK)
        idx = jnp.take_along_axis(cand_i, pos, axis=-1)
        gate = jax.nn.softmax(top_s, axis=-1).astype(hb.dtype)
        u = expert_u[idx]
        act = jax.nn.gelu(jnp.einsum('thkd,td->thk', u, hb), approximate=False)
        vv = expert_v[idx]
        return jnp.einsum('thk,thkd->td', gate * act, vv)

    out = lax.map(block, hb_all)
    return out.reshape(B, S, D)


def setup_inputs(seed: int = 0) -> dict:
    key = jax.random.key(seed)
    ks = iter(jax.random.split(key, 40))
    f32 = jnp.float32
    nrm = lambda shape, scale: jax.random.normal(next(ks), shape, f32) * scale
    gain = lambda shape: 1.0 + 0.02 * jax.random.normal(next(ks), shape, f32)
    L = DEPTH
    x = jax.random.normal(next(ks), (BATCH, SEQ, D_MODEL), f32)
    offset = jax.random.randint(next(ks), (BATCH, 1), 0, 64, dtype=jnp.int32) * CHUNK
    positions = offset + jnp.arange(SEQ, dtype=jnp.int32)[None, :]
    return {
        'x': x,
        'positions': positions,
        'g_mix': gain((L, D_MODEL)),
        'w_in': nrm((L, D_MODEL, IN_COLS), D_MODEL ** -0.5),
        'rwkv_mu': jax.random.uniform(next(ks), (L, RWKV_COLS), f32),
        'g_cq': gain((L, MLA_Q_RANK)),
        'g_ckv': gain((L, MLA_KV_RANK)),
        'w_uq': nrm((L, MLA_Q_RANK, MLA_HEADS * MLA_QK), MLA_Q_RANK ** -0.5),
        'w_uk': nrm((L, MLA_KV_RANK, MLA_HEADS * MLA_NOPE), MLA_KV_RANK ** -0.5),
        'w_uv': nrm((L, MLA_KV_RANK, MLA_HEADS * MLA_V), MLA_KV_RANK ** -0.5),
        'g_qnorm': gain((L, MLA_QK)),
        'g_knorm': gain((L, MLA_QK)),
        'g_attn_out': gain((L, MLA_HEADS * MLA_V)),
        'w0': -6.0 + 5.0 * jax.random.uniform(next(ks), (L, RWKV_DIM), f32),
        'w2': nrm((L, W_LORA, RWKV_DIM), 0.1),
        'a0': nrm((L, RWKV_DIM), 0.1),
        'a2': nrm((L, A_LORA, RWKV_DIM), 0.1),
        'g2': nrm((L, G_LORA, RWKV_DIM), G_LORA ** -0.5),
        'k_k': 0.85 + nrm((L, RWKV_DIM), 0.05),
        'k_a': 1.0 + nrm((L, RWKV_DIM), 0.05),
        'r_k': nrm((L, RWKV_HEADS, RWKV_HEAD), 0.1),
        'ln_x_w': gain((L, RWKV_DIM)),
        'ln_x_b': nrm((L, RWKV_DIM), 0.02),
        'w_o': nrm((L, MIX_WIDTH, D_MODEL), MIX_WIDTH ** -0.5),
        'g_ffn': gain((L, D_MODEL)),
        'w_pq': nrm((L, D_MODEL, PEER_HEADS * PEER_DQ), D_MODEL ** -0.5),
        'sub_keys': nrm((L, PEER_HEADS, 2, N_KEYS, PEER_DHALF), PEER_DHALF ** -0.5),
        'expert_u': nrm((L, N_EXPERTS, D_MODEL), D_MODEL ** -0.5),
        'expert_v': nrm((L, N_EXPERTS, D_MODEL), PEER_HEADS ** -0.5),
    }


def reference(x, positions, g_mix, w_in, rwkv_mu, g_cq, g_ckv, w_uq, w_uk, w_uv, g_qnorm, g_knorm,
              g_attn_out, w0, w2, a0, a2, g2, k_k, k_a, r_k, ln_x_w, ln_x_b, w_o, g_ffn, w_pq,
              sub_keys, expert_u, expert_v):
    for l in range(DEPTH):
        h = rms_norm(x, g_mix[l])
        p = h @ w_in[l]
        y_a = mla_group(p[..., :MLA_COLS], positions, g_cq[l], g_ckv[l], w_uq[l], w_uk[l], w_uv[l],
                        g_qnorm[l], g_knorm[l], g_attn_out[l])
        y_b = rwkv7_group(p[..., MLA_COLS:], rwkv_mu[l], w0[l], w2[l], a0[l], a2[l], g2[l], k_k[l], k_a[l],
                          r_k[l], ln_x_w[l], ln_x_b[l])
        x = x + jnp.concatenate([y_a, y_b], axis=-1) @ w_o[l]
        x = x + peer_ffn(rms_norm(x, g_ffn[l]), w_pq[l], sub_keys[l], expert_u[l], expert_v[l])
    return x
```

```python
import math
import numpy as np
from contextlib import ExitStack
import concourse.bass as bass
import concourse.mybir as mybir
from concourse.bass_utils import run_bass_kernel_spmd

F32 = mybir.dt.float32
BF16 = mybir.dt.bfloat16
I32 = mybir.dt.int32
U32 = mybir.dt.uint32
AF = mybir.ActivationFunctionType
ALU = mybir.AluOpType
AX = mybir.AxisListType

EPS = 1e-6
NV = 112


class Prog:
    ENG = ['pe', 'act', 'dve', 'pool', 'sp']
    NDS = 8

    def __init__(self, nc, stack):
        self.nc = nc
        self.sem = {e: stack.enter_context(nc.semaphore("s_" + e)) for e in self.ENG}
        self.cnt = {e: 0 for e in self.ENG}
        self.dsem = {q: [stack.enter_context(nc.semaphore("d_%s%d" % (q, i))) for i in range(self.NDS)]
                     for q in ['sp', 'act', 'pool']}
        self.dcnt = {q: 0 for q in self.dsem}
        self.waited = {e: {} for e in self.ENG}
        self.ops = {e: [] for e in self.ENG}
        self.last_w = {}
        self.rd = {}
        self.all_dma = []
        self.nins = 0

    def _deps(self, eng, reads, writes):
        deps = []
        for r in reads:
            t = self.last_w.get(r)
            if t is not None:
                deps.append(t)
        for w in writes:
            t = self.last_w.get(w)
            if t is not None:
                deps.append(t)
            deps.extend(self.rd.get(w, ()))
        wd = self.waited[eng]
        best = {}
        for (sem, val, key, teng) in deps:
            if teng == eng and eng == 'pe':
                continue
            if wd.get(key, 0) >= val:
                continue
            if best.get(key, (None, 0))[1] < val:
                best[key] = (sem, val)
        waits = []
        for key, (sem, val) in best.items():
            wd[key] = val
            waits.append((sem, val))
        return waits

    def _commit(self, tok, reads, writes):
        for w in writes:
            self.last_w[w] = tok
            self.rd[w] = []
        for r in reads:
            if r in writes:
                continue
            self.rd.setdefault(r, []).append(tok)

    def op(self, eng, fn, reads=(), writes=()):
        waits = self._deps(eng, reads, writes)
        self.cnt[eng] += 1
        tok = (self.sem[eng], self.cnt[eng], 'e_' + eng, eng)
        self.ops[eng].append((waits, fn, self.sem[eng], 1))
        self._commit(tok, reads, writes)
        self.nins += 1
        return tok

    def dma(self, q, fn, reads=(), writes=()):
        waits = self._deps(q, reads, writes)
        i = self.dcnt[q]
        self.dcnt[q] += 1
        slot = i % self.NDS
        val = 16 * (i // self.NDS + 1)
        sem = self.dsem[q][slot]
        key = 'd_%s%d' % (q, slot)
        if val > 16 and self.waited[q].get(key, 0) < val - 16:
            waits.append((sem, val - 16))
            self.waited[q][key] = val - 16
        tok = (sem, val, key, 'dma_' + q)
        self.ops[q].append((waits, fn, sem, 16))
        self._commit(tok, reads, writes)
        self.all_dma.append(tok)
        self.nins += 1
        return tok

    def wait_all_dma(self, eng='sp'):
        best = {}
        for (sem, val, key, _) in self.all_dma:
            if best.get(key, (None, 0))[1] < val:
                best[key] = (sem, val)
        waits = []
        for key, (sem, val) in best.items():
            if self.waited[eng].get(key, 0) < val:
                waits.append((sem, val))
                self.waited[eng][key] = val
        self.ops[eng].append((waits, None, None, 0))
        self.all_dma = []

    def emit(self):
        nc = self.nc
        ops = self.ops
        self.wait_all_dma('sp')
        with nc.Block() as block:
            def run(e, lst):
                for (waits, fn, sem, inc) in lst:
                    for (s, v) in waits:
                        e.wait_ge(s, v)
                    if fn is not None:
                        fn(e).then_inc(sem, inc)

            @block.tensor
            def _(e):
                run(e, ops['pe'])

            @block.scalar
            def _(e):
                run(e, ops['act'])

            @block.vector
            def _(e):
                run(e, ops['dve'])

            @block.gpsimd
            def _(e):
                run(e, ops['pool'])

            @block.sync
            def _(e):
                run(e, ops['sp'])
        self.ops = {e: [] for e in self.ENG}


class K:
    def __init__(self, P):
        self.P = P

    def mm(self, out, lhsT, rhs, start, stop, rd, wr):
        self.P.op('pe', lambda e: e.matmul(out, lhsT=lhsT, rhs=rhs, start=start, stop=stop), rd, wr)

    def tr(self, out, in_, ident, rd, wr):
        self.P.op('pe', lambda e: e.transpose(out, in_, ident), rd, wr)

    def act(self, out, in_, func, rd, wr, **kw):
        self.P.op('act', lambda e: e.activation(out=out, in_=in_, func=func, **kw), rd, wr)

    def tt(self, eng, out, in0, in1, op, rd, wr):
        self.P.op(eng, lambda e: e.tensor_tensor(out=out, in0=in0, in1=in1, op=op), rd, wr)

    def ts(self, eng, out, in0, s1, s2, op0, op1, rd, wr):
        if op1 is None:
            self.P.op(eng, lambda e: e.tensor_scalar(out=out, in0=in0, scalar1=s1, scalar2=None, op0=op0), rd, wr)
        else:
            self.P.op(eng, lambda e: e.tensor_scalar(out=out, in0=in0, scalar1=s1, scalar2=s2, op0=op0, op1=op1), rd, wr)

    def stt(self, out, in0, scalar, in1, op0, op1, rd, wr):
        self.P.op('dve', lambda e: e.scalar_tensor_tensor(out=out, in0=in0, scalar=scalar, in1=in1, op0=op0, op1=op1), rd, wr)

    def cp(self, eng, out, in_, rd, wr):
        if eng == 'act':
            self.P.op('act', lambda e: e.activation(out=out, in_=in_, func=AF.Copy), rd, wr)
        else:
            self.P.op(eng, lambda e: e.tensor_copy(out=out, in_=in_), rd, wr)

    def recip(self, out, in_, rd, wr):
        self.P.op('dve', lambda e: e.reciprocal(out=out, in_=in_), rd, wr)

    def memset(self, eng, ap, val, wr):
        self.P.op(eng, lambda e: e.memset(ap, val), (), wr)

    def dma(self, q, out, in_, rd, wr, slow=False):
        if slow:
            self.P.dma(q, lambda e: e.dma_start(out=out, in_=in_, allow_slow_non_contiguous=True), rd, wr)
        else:
            self.P.dma(q, lambda e: e.dma_start(out=out, in_=in_), rd, wr)


VC = {}
_c = 0
for _n, _w in [('g_mix', 8), ('g_cq', 3), ('g_ckv', 2), ('g_q', 1), ('g_qp', 1), ('g_k', 1), ('g_kp', 1),
               ('g_ao', 8), ('mu', 14), ('w0', 4), ('a0', 4), ('k_k', 4), ('k_a', 4), ('r_k', 4),
               ('ln_w', 4), ('ln_b', 4), ('inv', 1), ('sgn', 1), ('iota16', 16)]:
    VC[_n] = _c
    _c += _w
assert _c <= NV


def build(S=2048, dbg=(), upto='all'):
    T = 2 * S
    NBLK = T // 512
    BPS = S // 512
    nc = bass.Bass("TRN2", target_bir_lowering=False)
    din = lambda n, s, d: nc.dram_tensor(n, s, d, kind="ExternalInput").ap()

    def dscr(n, s, d):
        kind = "ExternalOutput" if n in dbg else "Internal"
        return nc.dram_tensor(n, s, d, kind=kind).ap()

    xT_d = din("xT", [1024, T], F32)
    x_d = din("x", [T, 1024], F32)
    pos_d = din("pos", [T], I32)
    vecs_d = din("vecs", [128, NV], F32)
    ident_d = din("ident", [128, 128], F32)
    w_in_d = din("w_in", [1024, 2464], F32)
    w_krp_d = din("w_krp", [1024, 96], F32)
    w_uq_d = din("w_uq", [384, 768], F32)
    w_uqp_d = din("w_uqp", [384, 768], F32)
    w_uk_d = din("w_uk", [256, 512], F32)
    w_uv_d = din("w_uv", [256, 512], F32)
    w2_d = din("w2", [64, 512], F32)
    a2_d = din("a2", [64, 512], F32)
    g2_d = din("g2", [128, 512], F32)
    w_o_d = din("w_o", [1024, 1024], F32)
    g_ffn_d = din("g_ffn", [1024], F32)
    w_pq_d = din("w_pq", [1024, 2048], F32)
    keysT_d = din("keysT", [16, 128, 128], F32)
    euv_d = din("expert_uv", [16384, 2048], F32)
    out_d = nc.dram_tensor("out", [T, 1024], F32, kind="ExternalOutput").ap()

    QT_d = dscr("QT", [2, 8, 96, S], BF16)
    KT_d = dscr("KT", [2, 8, 96, S], BF16)
    VA_d = dscr("VA", [T // 128, 128, 8, 128], BF16)
    KKf_d = dscr("KKf", [512, T], BF16)
    Rf_d = dscr("Rf", [512, T], BF16)
    Wf_d = dscr("Wf", [512, T], F32)
    NBt_d = dscr("NBt", [T, 512], BF16)
    KPt_d = dscr("KPt", [T, 512], BF16)
    Vt_d = dscr("Vt", [T, 512], BF16)
    Gf_d = dscr("Gf", [512, T], F32)
    Bf_d = dscr("Bf", [512, T], F32)
    Yt_d = dscr("Yt", [T, 512], BF16)
    YA_d = dscr("YA", [8, 64, T], BF16)
    YB_d = dscr("YB", [512, T], BF16)
    EUVb_d = dscr("EUVb", [16384, 2048], BF16)
    FMall_d = dscr("FMall", [4, 512, T], BF16)
    KTf_d, RTf_d, BTf_d, KKTf_d = FMall_d[0], FMall_d[1], FMall_d[2], FMall_d[3]
    TMall_d = dscr("TMall", [T, 3, 512], BF16)
    KTt_d, BHt_d, KHt_d = TMall_d[:, 0, :], TMall_d[:, 1, :], TMall_d[:, 2, :]
    WCf_d = dscr("WCf", [512, T // 64], F32)

    with ExitStack() as top:
        P = Prog(nc, top)
        k = K(P)
        psum = [top.enter_context(nc.psum_tensor("ps%d" % i, [128, 512], F32)) for i in range(8)]
        pctr = [0]

        def nps():
            i = pctr[0] % 8
            pctr[0] += 1
            return psum[i], ('ps', i)

        uid = [0]

        def un(n):
            uid[0] += 1
            return "%s_u%d" % (n, uid[0])

        csb = lambda n, s, d: top.enter_context(nc.sbuf_tensor(un(n), s, d))
        vecs = csb("vecs", [128, NV], F32)
        ident_b = csb("ident_b", [128, 128], BF16)
        ones_b = csb("ones_b", [128, 128], BF16)
        bones_b = csb("bones_b", [128, 128], BF16)
        omka = csb("omka", [128, 4], F32)
        epsc = csb("epsc", [128, 1], F32)
        lnxe = csb("lnxe", [128, 1], F32)
        k.dma('sp', vecs[:], vecs_d, (), ['vecs'])
        vcol = lambda n, j=0, lo=0, hi=128: vecs[lo:hi, VC[n] + j:VC[n] + j + 1]

        wsb = lambda n, s, d: top.enter_context(nc.sbuf_tensor(un(n), s, d))
        with ExitStack() as st:
            sb = lambda n, s, d: st.enter_context(nc.sbuf_tensor(un(n), s, d))
            stg = [sb("stg%d" % i, [128, 2464], F32) for i in range(2)]
            k.dma('sp', stg[0][:, 0:128], ident_d, (), ['stg0'])
            k.cp('dve', ident_b[:], stg[0][:, 0:128], ['stg0'], ['ident'])
            k.memset('pool', ones_b[:], 1.0, ['ones'])
            k.memset('pool', bones_b[:], 0.0, ['bones'])
            k.memset('pool', bones_b[0:64, 0:64], 1.0, ['bones'])
            k.memset('pool', bones_b[64:128, 64:128], 1.0, ['bones'])
            k.ts('dve', omka[:], vecs[:, VC['k_a']:VC['k_a'] + 4], -1.0, 1.0, ALU.mult, ALU.add, ['vecs'], ['omka'])
            k.memset('pool', epsc[:], EPS, ['epsc'])
            k.memset('pool', lnxe[:], 64e-5, ['lnxe'])
            P.emit()

        si = [1]

        def load_w(stg, dst, src, rows, cols, gname, gj):
            s_ = stg[si[0] % 2]
            key = 'stg%d' % (si[0] % 2)
            si[0] += 1
            k.dma('sp', s_[0:rows, 0:cols], src, (), [key])
            if gname is None:
                k.cp('dve', dst, s_[0:rows, 0:cols], [key], ['wts'])
            else:
                k.ts('dve', dst, s_[0:rows, 0:cols], vcol(gname, gj, 0, rows), None, ALU.mult, None,
                     [key, 'vecs'], ['wts'])

        xT_v = xT_d.rearrange("(kc p) t -> p kc t", p=128)

        def phase_inproj(mode):
            with ExitStack() as st:
                sb = lambda n, s, d: st.enter_context(nc.sbuf_tensor(un(n), s, d))
                ncol = 672 if mode == 'mla' else 1792
                c0 = 0 if mode == 'mla' else 672
                w_in_b = sb("w_in_b", [128, 8, ncol], BF16)
                if mode == 'mla':
                    w_krp_b = sb("w_krp_b", [128, 8, 96], BF16)
                    w_uq_b = sb("w_uq_b", [128, 3, 768], BF16)
                    w_uqp_b = sb("w_uqp_b", [128, 3, 768], BF16)
                    w_uk_b = sb("w_uk_b", [128, 2, 512], BF16)
                    w_uv_b = sb("w_uv_b", [128, 2, 512], BF16)
                else:
                    w2a2_b = sb("w2a2_b", [128, 512], BF16)
                    g2_b = sb("g2_b", [128, 512], BF16)
                with ExitStack() as st2:
                    stg = [st2.enter_context(nc.sbuf_tensor(un("stgw%d" % i), [128, 1792], F32)) for i in range(2)]
                    for kc in range(8):
                        load_w(stg, w_in_b[:, kc, :], w_in_d[kc * 128:(kc + 1) * 128, c0:c0 + ncol], 128, ncol, 'g_mix', kc)
                    if mode == 'mla':
                        for kc in range(8):
                            load_w(stg, w_krp_b[:, kc, :], w_krp_d[kc * 128:(kc + 1) * 128, :], 128, 96, 'g_mix', kc)
                        for kc in range(3):
                            load_w(stg, w_uq_b[:, kc, :], w_uq_d[kc * 128:(kc + 1) * 128, :], 128, 768, 'g_cq', kc)
                            load_w(stg, w_uqp_b[:, kc, :], w_uqp_d[kc * 128:(kc + 1) * 128, :], 128, 768, 'g_cq', kc)
                        for kc in range(2):
                            load_w(stg, w_uk_b[:, kc, :], w_uk_d[kc * 128:(kc + 1) * 128, :], 128, 512, 'g_ckv', kc)
                            load_w(stg, w_uv_b[:, kc, :], w_uv_d[kc * 128:(kc + 1) * 128, :], 128, 512, 'g_ckv', kc)
                    else:
                        s_ = stg[si[0] % 2]; key = 'stg%d' % (si[0] % 2); si[0] += 1
                        k.dma('sp', s_[0:64, 0:512], w2_d, (), [key])
                        k.dma('sp', s_[64:128, 0:512], a2_d, (), [key])
                        k.cp('dve', w2a2_b[:], s_[:, 0:512], [key], ['wts'])
                        load_w(stg, g2_b[:], g2_d, 128, 512, None, 0)
                    P.emit()

                xs_t = sb("xs", [128, 8, 512], F32)
                xb = sb("xb", [128, 8, 512], BF16)
                xsq = sb("xsq", [128, 8, 512], BF16)
                rstd_x = sb("rstd_x", [128, 512], F32)
                tmpa = sb("tmpa", [128, 512], F32)

                def rstd_from_ps(ps_t, pk, n, scale, dst, rows=128):
                    k.act(tmpa[0:rows, :], ps_t[0:rows, :], AF.Ln, [pk], ['tmpa'], scale=scale, bias=epsc[0:rows, :])
                    k.act(dst, tmpa[0:rows, :], AF.Exp, ['tmpa'], [n], scale=-0.5)

                def inproj(lhs_fn, m, dst, dk):
                    ps_t, pk = nps()
                    for kc in range(8):
                        k.mm(ps_t[0:m, :], lhs_fn(kc), xb[:, kc, :], kc == 0, kc == 7, ['wts', 'xb'], [pk])
                    k.tt('dve', dst, ps_t[0:m, :], rstd_x[0:m, :], ALU.mult, [pk, 'rstd_x'], [dk])

                def load_x(t0):
                    k.dma('sp', xs_t[:], xT_v[:, :, t0:t0 + 512], (), ['xs'])
                    k.cp('dve', xb[:], xs_t[:], ['xs'], ['xb'])
                    k.act(xsq[:], xs_t[:], AF.Square, ['xs'], ['xsq'])
                    ps_t, pk = nps()
                    for kc in range(8):
                        k.mm(ps_t[:], ones_b[:], xsq[:, kc, :], kc == 0, kc == 7, ['ones', 'xsq'], [pk])
                    rstd_from_ps(ps_t, pk, 'rstd_x', 1.0 / 1024, rstd_x[:])

                if mode == 'mla':
                    cq = sb("cq", [128, 3, 512], F32)
                    ckv = sb("ckv", [128, 2, 512], F32)
                    csq = sb("csq", [128, 3, 512], BF16)
                    cqn = sb("cqn", [128, 3, 512], BF16)
                    ckvn = sb("ckvn", [128, 2, 512], BF16)
                    rstd_c = sb("rstd_c", [128, 512], F32)
                    kr = sb("kr", [96, 512], F32)
                    krp = sb("krp", [96, 512], F32)
                    kcat = sb("kcat", [96, 512], F32)
                    hsq2 = [sb("hsq%d" % i, [96, 512], BF16) for i in range(2)]
                    rh2 = [sb("rh%d" % i, [96, 512], F32) for i in range(2)]
                    hn2 = [sb("hn%d" % i, [96, 512], F32) for i in range(2)]
                    hpn2 = [sb("hpn%d" % i, [96, 512], F32) for i in range(2)]
                    tmph2 = [sb("tmph%d" % i, [96, 512], F32) for i in range(2)]
                    qkT = [sb("qkT%d" % i, [96, 512], BF16) for i in range(2)]
                    vaug = sb("vaug", [128, 4, 8, 128], BF16)
                    posi = sb("posi", [96, 512], I32)
                    ang = sb("ang", [96, 512], F32)
                    rr = sb("rr", [96, 512], F32)
                    kki = sb("kki", [96, 512], I32)
                    Ct = sb("Ct", [96, 512], F32)
                    SSt = sb("SSt", [96, 512], F32)
                    k.memset('pool', vaug[:, :, :, 64:128], 1.0, ['vaug1'])
                    TWO_PI = 2.0 * math.pi
                    R = slice(64, 96)
                    for blk in range(NBLK):
                        seq = blk // BPS
                        t0 = blk * 512
                        ts_ = t0 - seq * S
                        load_x(t0)
                        k.dma('sp', posi[R, :], pos_d[t0:t0 + 512].partition_broadcast(32), (), ['posi'])
                        k.cp('dve', ang[R, :], posi[R, :], ['posi'], ['ang'])
                        k.ts('dve', ang[R, :], ang[R, :], vcol('inv', 0, 64, 96), None, ALU.mult, None, ['ang', 'vecs'], ['ang'])
                        for which, dst in (('sin', SSt), ('cos', Ct)):
                            off = 0.0 if which == 'sin' else math.pi / 2
                            k.ts('dve', rr[R, :], ang[R, :], off, 1.0 / TWO_PI, ALU.add, ALU.mult, ['ang'], ['rr'])
                            k.cp('dve', kki[R, :], rr[R, :], ['rr'], ['kki'])
                            k.cp('dve', rr[R, :], kki[R, :], ['kki'], ['rr'])
                            k.stt(dst[R, :], rr[R, :], -TWO_PI, ang[R, :], ALU.mult, ALU.add, ['rr', 'ang'], [which])
                            k.ts('dve', dst[R, :], dst[R, :], off, None, ALU.add, None, [which], [which])
                            k.ts('dve', dst[R, :], dst[R, :], math.pi, -math.pi, ALU.min, ALU.max, [which], [which])
                            k.act(dst[R, :], dst[R, :], AF.Sin, [which], [which])
                        k.ts('dve', SSt[R, :], SSt[R, :], vcol('sgn', 0, 64, 96), None, ALU.mult, None, ['sin', 'vecs'], ['sin'])

                        for c in range(3):
                            inproj(lambda kc, c=c: w_in_b[:, kc, c * 128:(c + 1) * 128], 128, cq[:, c, :], 'cq')
                        for c in range(2):
                            inproj(lambda kc, c=c: w_in_b[:, kc, 384 + c * 128:384 + (c + 1) * 128], 128, ckv[:, c, :], 'ckv')
                        inproj(lambda kc: w_in_b[:, kc, 576:672], 96, kr[:, :], 'kr')
                        inproj(lambda kc: w_krp_b[:, kc, :], 96, krp[:, :], 'krp')
                        for (src, sk_, n, dstn, dk) in ((cq, 'cq', 3, cqn, 'cqn'), (ckv, 'ckv', 2, ckvn, 'ckvn')):
                            k.act(csq[:, 0:n, :], src[:, 0:n, :], AF.Square, [sk_], ['csq'])
                            ps_t, pk = nps()
                            for c in range(n):
                                k.mm(ps_t[:], ones_b[:], csq[:, c, :], c == 0, c == n - 1, ['ones', 'csq'], [pk])
                            rstd_from_ps(ps_t, pk, 'rstd_c', 1.0 / (128 * n), rstd_c[:])
                            k.tt('dve', dstn[:, 0:n, :], src[:, 0:n, :],
                                 rstd_c[:].unsqueeze(1).broadcast_to([128, n, 512]), ALU.mult, [sk_, 'rstd_c'], [dk])
                        k.cp('act', kcat[R, :], kr[R, :], ['kr'], ['kcat_r'])
                        for h in range(8):
                            for which in ('q', 'k'):
                                hb = (h * 2 + (which == 'k')) % 2
                                hsq, rh, hn, hpn, tmph = hsq2[hb], rh2[hb], hn2[hb], hpn2[hb], tmph2[hb]
                                HSQ, RH, HN, HPN, TMPH = ('hsq', hb), ('rh', hb), ('hn', hb), ('hpn', hb), ('tmph', hb)
                                ps_t, pk = nps()
                                if which == 'q':
                                    for kc in range(3):
                                        k.mm(ps_t[0:96, :], w_uq_b[:, kc, h * 96:(h + 1) * 96], cqn[:, kc, :], kc == 0, kc == 2, ['wts', 'cqn'], [pk])
                                    ps2, pk2 = nps()
                                    for kc in range(3):
                                        k.mm(ps2[0:96, :], w_uqp_b[:, kc, h * 96:(h + 1) * 96], cqn[:, kc, :], kc == 0, kc == 2, ['wts', 'cqn'], [pk2])
                                    src_ap, srck = ps_t, [pk]
                                    k.act(hsq[:, :], ps_t[0:96, :], AF.Square, [pk], [HSQ])
                                    part_ap, partk = ps2, [pk2]
                                    gn, gpn = 'g_q', 'g_qp'
                                else:
                                    for kc in range(2):
                                        k.mm(ps_t[0:64, :], w_uk_b[:, kc, h * 64:(h + 1) * 64], ckvn[:, kc, :], kc == 0, kc == 1, ['wts', 'ckvn'], [pk])
                                    k.cp('act', kcat[0:64, :], ps_t[0:64, :], [pk], ['kcat_n'])
                                    src_ap, srck = kcat, ['kcat_n', 'kcat_r']
                                    k.act(hsq[:, :], kcat[:, :], AF.Square, srck, [HSQ])
                                    part_ap, partk = krp, ['krp']
                                    gn, gpn = 'g_k', 'g_kp'
                                ps3, pk3 = nps()
                                k.mm(ps3[0:96, :], ones_b[0:96, 0:96], hsq[:, :], True, True, ['ones', HSQ], [pk3])
                                k.act(tmph[:, :], ps3[0:96, :], AF.Ln, [pk3], [TMPH], scale=1.0 / 96, bias=epsc[0:96, :])
                                k.act(rh[:, :], tmph[:, :], AF.Exp, [TMPH], [RH], scale=-0.5)
                                oi = (h * 2 + (which == 'k')) % 2
                                ot, ok = qkT[oi], 'qkT%d' % oi
                                k.stt(ot[0:64, :], src_ap[0:64, :], vcol(gn, 0, 0, 64), rh[0:64, :], ALU.mult, ALU.mult,
                                      srck + [RH, 'vecs'], [ok])
                                k.stt(hn[R, :], src_ap[R, :], vcol(gn, 0, 64, 96), rh[R, :], ALU.mult, ALU.mult,
                                      srck + [RH, 'vecs'], [HN])
                                k.stt(hpn[R, :], part_ap[R, :], vcol(gpn, 0, 64, 96), rh[R, :], ALU.mult, ALU.mult,
                                      partk + [RH, 'vecs'], [HPN])
                                k.tt('pool', hn[R, :], hn[R, :], Ct[R, :], ALU.mult, [HN, 'cos'], [HN])
                                k.tt('dve', hpn[R, :], hpn[R, :], SSt[R, :], ALU.mult, [HPN, 'sin'], [HPN])
                                k.tt('pool', ot[R, :], hn[R, :], hpn[R, :], ALU.add, [HN, HPN], [ok])
                                dst_d = QT_d if which == 'q' else KT_d
                                k.dma('sp', dst_d[seq, h, :, ts_:ts_ + 512], ot[:, :], [ok], [])
                        for tt_ in range(4):
                            ps_t, pk = nps()
                            for kc in range(2):
                                k.mm(ps_t[:], ckvn[:, kc, tt_ * 128:(tt_ + 1) * 128], w_uv_b[:, kc, :], kc == 0, kc == 1, ['ckvn', 'wts'], [pk])
                            k.cp('act', vaug[:, tt_, :, 0:64], ps_t[:].rearrange("p (h d) -> p h d", d=64), [pk], ['vaug'])
                        k.dma('sp', VA_d[blk * 4:(blk + 1) * 4].rearrange("t p h d -> p t h d"), vaug[:], ['vaug', 'vaug1'], [])
                else:
                    pst = sb("pst", [128, 14, 513], F32)
                    carry = sb("carry", [128, 14, 1], F32)
                    dd = sb("dd", [128, 512], F32)
                    thal = sb("thal", [128, 512], BF16)
                    sgl = sb("sgl", [128, 512], BF16)
                    sig = sb("sig", [128, 512], F32)
                    dec = sb("dec", [128, 512], F32)
                    aa = sb("aa", [128, 512], F32)
                    gst = sb("gst", [128, 512], F32)
                    kkr = sb("kkr", [128, 512], F32)
                    ksq = sb("ksq", [128, 512], BF16)
                    rn = sb("rn", [128, 512], F32)
                    kkn32 = sb("kkn32", [128, 512], F32)
                    fb = [sb("fb%d" % i, [128, 512], BF16) for i in range(8)]
                    tq = sb("tq", [128, 512], F32)
                    kp32 = sb("kp32", [128, 512], F32)
                    rkk = sb("rkk", [128, 512], BF16)
                    bon = sb("bon", [128, 512], F32)
                    tokst = [sb("tokst%d" % i, [128, 4, 512], BF16) for i in range(4)]
                    lw = sb("lw", [128, 512], F32)
                    csm = sb("csm", [128, 512], F32)
                    eneg = sb("eneg", [128, 512], F32)
                    epos = sb("epos", [128, 512], F32)
                    eprev = sb("eprev", [128, 512], F32)
                    ehat = sb("ehat", [128, 512], F32)
                    wcs = sb("wcs", [128, 8], F32)
                    rmask = sb("rmask", [128, 512], F32)
                    k.memset('pool', rmask[:], 1.0, ['rmask'])
                    k.memset('pool', rmask[:].rearrange("p (c t) -> p c t", t=64)[:, :, 0:1], 0.0, ['rmask'])
                    fbi = [0]

                    def fbuf():
                        i = fbi[0] % 8
                        fbi[0] += 1
                        return fb[i], 'fb%d' % i

                    pmk = lambda c: ('pm', c)
                    for blk in range(NBLK):
                        seq = blk // BPS
                        t0 = blk * 512
                        load_x(t0)
                        if seq * BPS == blk:
                            k.memset('pool', carry[:], 0.0, ['carry'])
                        for c in range(14):
                            k.cp('pool', pst[:, c, 0:1], carry[:, c, :], ['carry'], [pmk(c)])
                            inproj(lambda kc, c=c: w_in_b[:, kc, c * 128:(c + 1) * 128], 128, pst[:, c, 1:513], pmk(c))
                            k.tt('pool', dd[:], pst[:, c, 0:512], pst[:, c, 1:513], ALU.subtract, [pmk(c)], ['dd'])
                            k.cp('pool', carry[:, c, :], pst[:, c, 512:513], [pmk(c)], ['carry'])
                            k.stt(pst[:, c, 1:513], dd[:], vcol('mu', c), pst[:, c, 1:513], ALU.mult, ALU.add,
                                  ['dd', 'vecs', pmk(c), 'carry'], [pmk(c)])
                        pm = lambda c: pst[:, c, 1:513]
                        pmr = lambda c, lo, hi: pst[lo:hi, c, 1:513]
                        k.act(thal[0:64, :], pmr(12, 0, 64), AF.Tanh, [pmk(12)], ['thal'])
                        k.cp('act', thal[64:128, :], pmr(12, 64, 128), [pmk(12)], ['thal'])
                        k.act(sgl[:], pm(13), AF.Sigmoid, [pmk(13)], ['sgl'])
                        for c in range(4):
                            cs = slice(c * 128, (c + 1) * 128)
                            ps_t, pk = nps()
                            k.mm(ps_t[:], w2a2_b[0:64, cs], thal[0:64, :], True, True, ['wts', 'thal'], [pk])
                            k.act(sig[:], ps_t[:], AF.Sigmoid, [pk, 'vecs'], ['sig'], bias=vcol('w0', c))
                            ps_t, pk = nps()
                            k.mm(ps_t[:], w2a2_b[64:128, cs], thal[64:128, :], True, True, ['wts', 'thal'], [pk])
                            k.act(aa[:], ps_t[:], AF.Sigmoid, [pk, 'vecs'], ['aa'], bias=vcol('a0', c))
                            ps_t, pk = nps()
                            k.mm(ps_t[:], g2_b[:, cs], sgl[:], True, True, ['wts', 'sgl'], [pk])
                            k.cp('act', gst[:], ps_t[:], [pk], ['gst'])
                            k.dma('sp', Gf_d[cs, t0:t0 + 512], gst[:], ['gst'], [])
                            k.ts('dve', kkr[:], pm(4 + c), vcol('k_k', c), None, ALU.mult, None, [pmk(4 + c), 'vecs'], ['kkr'])
                            k.act(ksq[:], kkr[:], AF.Square, ['kkr'], ['ksq'])
                            ps_t, pk = nps()
                            k.mm(ps_t[:], bones_b[:], ksq[:], True, True, ['bones', 'ksq'], [pk])
                            k.ts('dve', rn[:], ps_t[:], 1e-24, None, ALU.max, None, [pk], ['rn'])
                            k.act(rn[:], rn[:], AF.Ln, ['rn'], ['rn'])
                            k.act(rn[:], rn[:], AF.Exp, ['rn'], ['rn'], scale=-0.5)
                            k.tt('dve', kkn32[:], kkr[:], rn[:], ALU.mult, ['kkr', 'rn'], ['kkn32'])
                            k.ts('dve', lw[:], sig[:], -math.exp(-0.5), None, ALU.mult, None, ['sig'], ['lw'])
                            P.op('dve', lambda e: e.tensor_tensor_scan(out=csm[:], data0=rmask[:], data1=lw[:], initial=0.0,
                                                                       op0=ALU.mult, op1=ALU.add), ['lw', 'rmask'], ['csm'])
                            cs3 = csm[:].rearrange("p (c t) -> p c t", t=64)
                            k.act(eneg[:], csm[:], AF.Exp, ['csm'], ['eneg'], scale=-1.0)
                            k.act(epos[:], csm[:], AF.Exp, ['csm'], ['epos'])
                            k.tt('dve', tq[:], csm[:], lw[:], ALU.subtract, ['csm', 'lw'], ['tq'])
                            k.act(eprev[:], tq[:], AF.Exp, ['tq'], ['eprev'])
                            k.tt('dve', tq[:].rearrange("p (c t) -> p c t", t=64), cs3[:, :, 63:64].broadcast_to([128, 8, 64]), cs3,
                                 ALU.subtract, ['csm', 'eprev'], ['tq'])
                            k.act(ehat[:], tq[:], AF.Exp, ['tq'], ['ehat'])
                            k.act(wcs[:].unsqueeze(2), cs3[:, :, 63:64], AF.Exp, ['csm'], ['wcs'])
                            k.dma('sp', WCf_d[cs, blk * 8:(blk + 1) * 8], wcs[:], ['wcs'], [])
                            f_kt, fk_kt = fbuf()
                            k.tt('dve', f_kt[:], kkn32[:], eprev[:], ALU.mult, ['kkn32', 'eprev'], [fk_kt])
                            k.dma('sp', KTf_d[cs, t0:t0 + 512], f_kt[:], [fk_kt], [])
                            f_r, fk_r = fbuf()
                            k.tt('dve', f_r[:], pm(c), epos[:], ALU.mult, [pmk(c), 'epos'], [fk_r])
                            k.dma('sp', RTf_d[cs, t0:t0 + 512], f_r[:], [fk_r], [])
                            k.tt('dve', kkn32[:], kkn32[:], aa[:], ALU.mult, ['kkn32', 'aa', fk_kt], ['kkn32'])
                            f_bt, fk_bt = fbuf()
                            k.tt('dve', f_bt[:], kkn32[:], eneg[:], ALU.mult, ['kkn32', 'eneg'], [fk_bt])
                            k.dma('sp', BTf_d[cs, t0:t0 + 512], f_bt[:], [fk_bt], [])
                            f_bh, fk_bh = fbuf()
                            k.tt('dve', f_bh[:], kkn32[:], ehat[:], ALU.mult, ['kkn32', 'ehat'], [fk_bh])
                            k.ts('dve', tq[:], aa[:], vcol('k_a', c), omka[:, c:c + 1], ALU.mult, ALU.add, ['aa', 'vecs', 'omka', 'ehat'], ['tq'])
                            k.tt('dve', kp32[:], pm(4 + c), tq[:], ALU.mult, [pmk(4 + c), 'tq'], ['kp32'])
                            f_kkt, fk_kkt = fbuf()
                            k.tt('dve', f_kkt[:], kp32[:], eneg[:], ALU.mult, ['kp32', 'eneg'], [fk_kkt])
                            k.dma('sp', KKTf_d[cs, t0:t0 + 512], f_kkt[:], [fk_kkt], [])
                            f_kh, fk_kh = fbuf()
                            k.tt('dve', f_kh[:], kp32[:], ehat[:], ALU.mult, ['kp32', 'ehat'], [fk_kh])
                            f_v, fk_v = fbuf()
                            k.cp('act', f_v[:], pm(8 + c), [pmk(8 + c)], [fk_v])
                            k.stt(rkk[:], pm(c), vcol('r_k', c), kp32[:], ALU.mult, ALU.mult, [pmk(c), 'vecs', 'kp32'], ['rkk'])
                            ps_t, pk = nps()
                            k.mm(ps_t[:], bones_b[:], rkk[:], True, True, ['bones', 'rkk'], [pk])
                            k.tt('dve', bon[:], ps_t[:], pm(8 + c), ALU.mult, [pk, pmk(8 + c)], ['bon'])
                            k.dma('sp', Bf_d[cs, t0:t0 + 512], bon[:], ['bon'], [])
                            for ai, (f_t, fk_t) in enumerate(((f_kt, fk_kt), (f_bh, fk_bh), (f_kh, fk_kh), (f_v, fk_v))):
                                ps_t, pk = nps()
                                psb = ps_t[:].bitcast(BF16)
                                for tt_ in range(4):
                                    k.tr(psb[:, tt_ * 128:(tt_ + 1) * 128], f_t[:, tt_ * 128:(tt_ + 1) * 128], ident_b[:], [fk_t, 'ident'], [pk])
                                k.cp('act' if ai % 2 == 0 else 'dve', tokst[ai][:, :, cs],
                                     psb[:, 0:512].rearrange("p (t f) -> p t f", f=128), [pk], [('tokst', ai)])
                        for ai, dd_ in enumerate((KTt_d, BHt_d, KHt_d, Vt_d)):
                            k.dma('sp', dd_[t0:t0 + 512, :].rearrange("(t p) f -> p t f", p=128), tokst[ai][:], [('tokst', ai)], [])
                P.emit()

        phase_inproj('mla')
        phase_inproj('rwkv')
        if upto == 'A':
            return nc

        def npsr(lo, hi, ctr=[0]):
            i = lo + ctr[0] % (hi - lo)
            ctr[0] += 1
            return psum[i], ('ps', i)

        with ExitStack() as st:
            sb = lambda n, s, d: st.enter_context(nc.sbuf_tensor(un(n), s, d))
            NT = S // 128
            qT = [sb("qT%d" % i, [96, S], BF16) for i in range(2)]
            kT = [sb("kT%d" % i, [96, S], BF16) for i in range(2)]
            Va = [sb("Va%d" % i, [128, NT, 128], BF16) for i in range(2)]
            pt = [sb("pt%d" % i, [128, 512], BF16) for i in range(4)]
            Lm = sb("Lm", [128, 64], BF16)
            Z = sb("Z", [128, 512], BF16)
            tmpb = sb("tmpb", [64, 512], F32)
            rsb = sb("rsb", [64, 512], F32)
            yo = [sb("yo%d" % i, [64, 512], BF16) for i in range(2)]
            k.memset('pool', Lm[:], 0.0, ['Lm'])
            k.memset('pool', Lm[0:64, :], 1.0 / 64, ['Lm'])
            k.memset('pool', Lm[64:65, :], EPS, ['Lm'])
            cin = [sb("cin%d" % i, [128, 2048], F32) for i in range(3)]
            cout = [sb("cout%d" % i, [128, 2048], BF16) for i in range(3)]
            cvi = [0]

            def convert_some(nchunks):
                for _ in range(nchunks):
                    ch = cvi[0]
                    if ch >= 128 + 2:
                        return
                    cvi[0] += 1
                    if ch < 128:
                        i = ch % 3
                        k.dma('sp', cin[i][:], euv_d[ch * 128:(ch + 1) * 128, :], (), [('cin', i)])
                    if ch >= 2:
                        c2 = ch - 2
                        i = c2 % 3
                        k.cp('dve', cout[i][:], cin[i][:], [('cin', i)], [('cout', i)])
                        k.dma('sp', EUVb_d[c2 * 128:(c2 + 1) * 128, :], cout[i][:], [('cout', i)], [])

            SC = 96 ** -0.5
            it = 0
            pti = [0]
            for seq in range(2):
                for h in range(8):
                    b_ = it % 2
                    it += 1
                    k.dma('sp', qT[b_][:], QT_d[seq, h], (), [('qT', b_)])
                    k.dma('sp', kT[b_][:], KT_d[seq, h], (), [('kT', b_)])
                    k.dma('sp', Va[b_][:], VA_d[seq * NT:(seq + 1) * NT, :, h, :].rearrange("t p d -> p t d"), (), [('Va', b_)])
                    convert_some(9)
                    for qb in range(S // 512):
                        po, pok = psum[6 + qb % 2], ('ps', 6 + qb % 2)
                        nkt = 4 * (qb + 1)
                        pend = []

                        def qk(kt):
                            j = kt - 4 * qb
                            c0 = max(0, 128 * j)
                            ps_t, pk = npsr(0, 6)
                            k.mm(ps_t[:, c0:512], kT[b_][:, kt * 128:(kt + 1) * 128], qT[b_][:, qb * 512 + c0:(qb + 1) * 512],
                                 True, True, [('kT', b_), ('qT', b_)], [pk])
                            i_ = pti[0] % 4
                            pti[0] += 1
                            p_, ptk = pt[i_], ('pt', i_)
                            k.act(p_[:, c0:512], ps_t[:, c0:512], AF.Exp, [pk], [ptk], scale=SC)
                            if j >= 0:
                                k.memset('pool', p_[64:128, c0:c0 + 64], 0.0, [ptk])
                            return (kt, c0, p_, ptk)

                        def pv(item):
                            kt, c0, p_, ptk = item
                            k.mm(po[:, c0:512], Va[b_][:, kt, :], p_[:, c0:512], kt == 0, kt == nkt - 1, [('Va', b_), ptk], [pok])

                        for kt in range(nkt):
                            pend.append(qk(kt))
                            if len(pend) > 2:
                                pv(pend.pop(0))
                        while pend:
                            pv(pend.pop(0))
                        k.act(Z[:], po[:], AF.Square, [pok], ['Z'])
                        ps_t, pk = npsr(0, 6)
                        k.mm(ps_t[0:64, :], Lm[:], Z[:], True, True, ['Lm', 'Z'], [pk])
                        k.act(tmpb[:], ps_t[0:64, :], AF.Ln, [pk], ['tmpb'])
                        k.act(rsb[:], tmpb[:], AF.Exp, ['tmpb'], ['rsb'], scale=-0.5)
                        y_, yk = yo[qb % 2], ('yo', qb % 2)
                        k.stt(y_[:], po[0:64, :], vecs[0:64, VC['g_ao'] + h:VC['g_ao'] + h + 1], rsb[:], ALU.mult, ALU.mult,
                              [pok, 'rsb', 'vecs'], [yk])
                        k.dma('sp', YA_d[h, :, seq * S + qb * 512:seq * S + (qb + 1) * 512], y_[:], [yk], [])
            P.emit()
        if upto == 'B':
            return nc

        w_oA = csb("w_oA", [64, 8, 1024], BF16)
        w_oB = csb("w_oB", [128, 4, 1024], BF16)
        w_pq_b = csb("w_pq_b", [128, 8, 2048], BF16)
        keys_b = csb("keys_b", [128, 16, 128], BF16)
        gffn = csb("gffn", [128, 1024], F32)
        MU_d = nc.dram_tensor("MU", [128, 256], F32, kind="ExternalInput").ap()
        ML_d = nc.dram_tensor("ML", [128, 128], F32, kind="ExternalInput").ap()
        with ExitStack() as st:
            sb = lambda n, s, d: st.enter_context(nc.sbuf_tensor(un(n), s, d))
            NCH = S // 64
            MU = sb("MUs", [128, 256], F32)
            ML = sb("MLs", [128, 128], F32)
            ident_f = sb("ident_fc", [128, 128], F32)
            k.dma('sp', MU[:], MU_d, (), ['MU'])
            k.dma('sp', ML[:], ML_d, (), ['ML'])
            k.cp('dve', ident_f[:], ident_b[:], ['ident'], ['ident_f'])
            FM = [sb("FM%d" % i, [128, 2, 4, 4, 128], BF16) for i in range(2)]
            TM = [sb("TM%d" % i, [128, 2, 3, 4, 128], BF16) for i in range(2)]
            TV = [sb("TV%d" % i, [128, 8, 64], BF16) for i in range(2)]
            WCt = sb("WCt", [128, 8, NCH], F32)
            for i in range(2):
                k.memset('pool', FM[i][:], 0.0, [('FM', i)])
                k.memset('pool', TM[i][:], 0.0, [('TM', i)])
            Sst = sb("Sst", [128, 8, 64], F32)
            Sb = sb("Sb", [128, 8, 64], BF16)
            k.memset('dve', Sst[:], 0.0, ['Sst'])
            k.memset('dve', Sb[:], 0.0, ['Sb'])
            stgd = [sb("stgd%d" % i, [128, 2048], F32) for i in range(2)]
            sdi = [0]

            def load_wd(dst, src, rows, cols, view=None):
                s_ = stgd[sdi[0] % 2]
                key = ('stgd', sdi[0] % 2)
                sdi[0] += 1
                sv = s_[0:rows, 0:cols] if view is None else view(s_)
                k.dma('sp', sv, src, (), [key])
                k.cp('act' if sdi[0] % 2 else 'dve', dst, sv, [key], ['wtsD'])

            for h in range(8):
                load_wd(w_oA[:, h, :], w_o_d[h * 64:(h + 1) * 64, :], 64, 1024)
            for c in range(4):
                load_wd(w_oB[:, c, :], w_o_d[512 + c * 128:512 + (c + 1) * 128, :], 128, 1024)
            for kc in range(8):
                load_wd(w_pq_b[:, kc, :], w_pq_d[kc * 128:(kc + 1) * 128, :], 128, 2048)
            for g4 in range(4):
                load_wd(keys_b[:, g4 * 4:(g4 + 1) * 4, :], keysT_d[g4 * 4:(g4 + 1) * 4].rearrange("a d n -> d a n"), 128, 512,
                        view=lambda s_: s_[:, 0:512].rearrange("p (a n) -> p a n", n=128))
            k.dma('sp', gffn[:], g_ffn_d.partition_broadcast(128), (), ['gffn'])
            ATB = sb("ATB", [128, 8, 256], BF16)
            AKB = sb("AKB", [128, 8, 256], BF16)
            AL = sb("AL", [128, 8, 128], BF16)
            MLv = [sb("MLv%d" % i, [128, 8, 256], BF16) for i in range(5)]
            AKV = sb("AKV", [128, 8, 64], BF16)
            X = [sb("X%d" % i, [128, 8, 192], BF16) for i in range(2)]
            GH = sb("GH", [128, 8, 192], BF16)
            PhiT = sb("PhiT", [128, 8, 128], BF16)
            OmT = sb("OmT", [128, 8, 128], BF16)
            PsY = sb("PsY", [128, 8, 128], F32)
            Yacc = [sb("Yacc%d" % i, [128, 8, 64], BF16) for i in range(2)]
            fm_src = (KTf_d, RTf_d, BTf_d, KKTf_d)
            tm_src = (KTt_d, BHt_d, KHt_d)
            dq = ['sp', 'pool']
            dqi = [0]

            def qn():
                dqi[0] += 1
                return dq[dqi[0] % 2]

            for b in range(2):
                for p in range(4):
                    k.dma(qn(), WCt[:, b * 4 + p, :], WCf_d[p * 128:(p + 1) * 128, b * NCH:(b + 1) * NCH], (), ['WCt'])

            def load_chunk(ci):
                bi = ci % 2
                for b in range(2):
                    tok = slice(b * S + ci * 64, b * S + (ci + 1) * 64)
                    for e in range(2):
                        er = slice(e * 64, (e + 1) * 64)
                        k.dma(qn(), FM[bi][er, b, :, :, e * 64:(e + 1) * 64].rearrange("q a p t -> q (a p) t"),
                              FMall_d[:, :, tok].rearrange("a (p q) t -> q (a p) t", q=128)[er], (), [('FM', bi)])
                        k.dma(qn(), TM[bi][er, b, :, :, e * 64:(e + 1) * 64].rearrange("t a p j -> t (a p) j"),
                              TMall_d[tok, :, :].rearrange("t a (p q) -> t (a p) q", q=128)[:, :, er], (), [('TM', bi)])
                        k.dma(qn(), TV[bi][er, b * 4:(b + 1) * 4, :],
                              Vt_d[tok, :].rearrange("t (p q) -> t p q", q=128)[:, :, er], (), [('TV', bi)])

            G8 = range(8)
            evi = [0]

            for ci in range(NCH):
                bi = ci % 2
                load_chunk(ci)
                fm, tm, tv = FM[bi], TM[bi], TV[bi]
                FMV = lambda g, a, fm=fm: fm[:, g // 4, a, g % 4, :]
                TMV = lambda g, a, tm=tm: tm[:, g // 4, a, g % 4, :]
                FMKR = lambda g, fm=fm: fm[:, g // 4, 0:2, g % 4, :]
                fk, tk_, vk = ('FM', bi), ('TM', bi), ('TV', bi)
                bank = lambda g: (psum[g], ('ps', g))
                for g in G8:
                    ps_t, pk = bank(g)
                    k.mm(ps_t[:, 0:256].rearrange("p (a t) -> p a t", a=2), FMV(g, 2), FMKR(g), True, True, [fk], [pk])
                    k.tt('dve', ATB[:, g, :], ps_t[:, 0:256], MU[:], ALU.mult, [pk, 'MU'], [('ATB', g)])
                for g in G8:
                    ps_t, pk = bank(g)
                    k.mm(ps_t[:, 0:256].rearrange("p (a t) -> p a t", a=2), FMV(g, 3), FMKR(g), True, True, [fk], [pk])
                    k.tt('dve', AKB[:, g, :], ps_t[:, 0:256], MU[:], ALU.mult, [pk, 'MU'], [('AKB', g)])
                for g in G8:
                    ps_t, pk = bank(g)
                    k.mm(ps_t[:, 0:128], FMV(g, 0), FMV(g, 2), True, True, [fk], [pk])
                    k.tt('dve', AL[:, g, :], ps_t[:, 0:128], ML[:], ALU.mult, [pk, 'ML'], [('AL', g)])
                for g in G8:
                    ps_t, pk = bank(g)
                    k.mm(ps_t[:, 0:64], AKB[:, g, 0:128], tv[:, g, :], True, True, [('AKB', g), vk], [pk])
                    k.cp('act', AKV[:, g, :], ps_t[:, 0:64], [pk], [('AKV', g)])
                for g in G8:
                    ps_t, pk = bank(g)
                    k.mm(ps_t[:, 0:128], ATB[:, g, 0:128], TMV(g, 0), True, True, [('ATB', g), tk_], [pk])
                    k.mm(ps_t[:, 128:192], ATB[:, g, 0:128], AKV[:, g, :], True, True, [('ATB', g), ('AKV', g)], [pk])
                    k.tt('dve', X[0][:, g, 0:128], TMV(g, 0), ps_t[:, 0:128], ALU.subtract, [pk, tk_], [('X0', g)])
                    k.tt('dve', X[0][:, g, 128:192], AKV[:, g, :], ps_t[:, 128:192], ALU.subtract, [pk, ('AKV', g)], [('X0', g)])
                xi = 0
                for lvl in range(5):
                    last = (lvl == 4)
                    for g in G8:
                        ps_t, pk = bank(g)
                        if lvl == 0:
                            Mk, Lk, mk_ = ATB[:, g, 0:128], AL[:, g, :], [('ATB', g), ('AL', g)]
                        else:
                            Mk, Lk, mk_ = MLv[lvl - 1][:, g, 0:128], MLv[lvl - 1][:, g, 128:256], [('MLv', lvl - 1, g)]
                        k.mm(ps_t[:, 0:128], Lk, Mk, True, True, mk_, [pk])
                        if not last:
                            k.mm(ps_t[:, 128:256], Mk, Lk, True, True, mk_, [pk])
                            k.cp('act', MLv[lvl][:, g, :], ps_t[:, 0:256], [pk], [('MLv', lvl, g)])
                        else:
                            k.cp('act', MLv[lvl][:, g, 0:128], ps_t[:, 0:128], [pk], [('MLv', lvl, g)])
                    for g in G8:
                        ps_t, pk = bank(g)
                        xs_, xd_ = X[xi], X[1 - xi]
                        k.mm(ps_t[:, 0:192], MLv[lvl][:, g, 0:128], xs_[:, g, :], True, True, [('MLv', lvl, g), ('X%d' % xi, g)], [pk])
                        if not last:
                            k.tt('dve', xd_[:, g, :], xs_[:, g, :], ps_t[:, 0:192], ALU.add, [pk, ('X%d' % xi, g)], [('X%d' % (1 - xi), g)])
                        else:
                            k.stt(GH[:, g, :], ps_t[:, 0:192], -1.0, xs_[:, g, :], ALU.mult, ALU.subtract, [pk, ('X%d' % xi, g)], [('GH', g)])
                    xi = 1 - xi
                for g in G8:
                    ps_t, pk = bank(g)
                    k.mm(ps_t[:, 0:128], GH[:, g, 0:128], TMV(g, 1), True, True, [('GH', g), tk_], [pk])
                    k.mm(ps_t[:, 128:256], GH[:, g, 0:128], ATB[:, g, 128:256], True, True, [('GH', g), ('ATB', g)], [pk])
                    k.stt(PhiT[:, g, :], ident_f[:], WCt[:, g, ci:ci + 1], ps_t[:, 0:128], ALU.mult, ALU.add, [pk, 'ident_f', 'WCt'], [('PhiT', g)])
                    k.tt('dve', OmT[:, g, :], ps_t[:, 128:256], FMV(g, 1), ALU.add, [pk, fk], [('OmT', g)])
                for g in G8:
                    ps_t, pk = bank(g)
                    k.mm(ps_t[:, 0:64], TMV(g, 1), GH[:, g, 128:192], True, False, [tk_, ('GH', g)], [pk])
                    k.mm(ps_t[:, 0:64], TMV(g, 2), tv[:, g, :], False, True, [tk_, vk], [pk])
                    k.mm(ps_t[:, 64:128], ATB[:, g, 128:256], GH[:, g, 128:192], True, False, [('ATB', g), ('GH', g)], [pk])
                    k.mm(ps_t[:, 64:128], AKB[:, g, 128:256], tv[:, g, :], False, True, [('AKB', g), vk], [pk])
                    k.cp('act', PsY[:, g, :], ps_t[:, 0:128], [pk], [('PsY', g)])
                ya = Yacc[ci % 2]
                yk = ('Yacc', ci % 2)
                for g in G8:
                    ps_t, pk = bank(g)
                    k.mm(ps_t[:, 0:64], OmT[:, g, :], Sb[:, g, :], True, True, [('OmT', g), ('Sb', g)], [pk])
                    k.mm(ps_t[:, 64:128], PhiT[:, g, :], Sb[:, g, :], True, True, [('PhiT', g), ('Sb', g)], [pk])
                    k.tt('dve', ya[:, g, :], ps_t[:, 0:64], PsY[:, g, 64:128], ALU.add, [pk, ('PsY', g)], [yk])
                    k.tt('dve', Sst[:, g, :], ps_t[:, 64:128], PsY[:, g, 0:64], ALU.add, [pk, ('PsY', g)], [('Sst', g)])
                    k.cp('act', Sb[:, g, :], Sst[:, g, :], [('Sst', g)], [('Sb', g)])
                for b in range(2):
                    tok = slice(b * S + ci * 64, b * S + (ci + 1) * 64)
                    for e in range(2):
                        er = slice(e * 64, (e + 1) * 64)
                        k.dma(qn(), Yt_d[tok, :].rearrange("t (p q) -> t p q", q=128)[:, :, er], ya[er, b * 4:(b + 1) * 4, :], [yk], [])
            P.emit()
        if upto == 'C':
            return nc

        with ExitStack() as st:
            sb = lambda n, s, d: st.enter_context(nc.sbuf_tensor(un(n), s, d))
            ytok = sb("ytok", [128, 4, 512], BF16)
            yb = sb("yb", [128, 512], BF16)
            cen = sb("cen", [128, 512], F32)
            sq = sb("sq", [128, 512], BF16)
            tmpc = sb("tmpc", [128, 512], F32)
            rs = sb("rs", [128, 512], F32)
            bfb = sb("bfb", [128, 512], F32)
            gfb = sb("gfb", [128, 512], F32)
            ybo = [sb("ybo%d" % i, [128, 512], BF16) for i in range(2)]
            for blk in range(NBLK):
                t0 = blk * 512
                k.dma('sp', ytok[:], Yt_d[t0:t0 + 512, :].rearrange("(t p) f -> p t f", p=128), (), ['ytok'])
                for c in range(4):
                    cs = slice(c * 128, (c + 1) * 128)
                    ps_t, pk = npsr(0, 8)
                    psb = ps_t[:].bitcast(BF16)
                    for tt_ in range(4):
                        k.tr(psb[:, tt_ * 128:(tt_ + 1) * 128], ytok[:, tt_, cs], ident_b[:], ['ytok', 'ident'], [pk])
                    k.cp('act', yb[:], psb[:, 0:512], [pk], ['yb'])
                    k.dma('sp', bfb[:], Bf_d[cs, t0:t0 + 512], (), ['bfb'])
                    k.dma('sp', gfb[:], Gf_d[cs, t0:t0 + 512], (), ['gfb'])
                    ps2, pk2 = npsr(0, 8)
                    k.mm(ps2[:], bones_b[:], yb[:], True, True, ['bones', 'yb'], [pk2])
                    k.stt(cen[:], ps2[:], -1.0 / 64, yb[:], ALU.mult, ALU.add, [pk2, 'yb'], ['cen'])
                    k.act(sq[:], cen[:], AF.Square, ['cen'], ['sq'])
                    ps3, pk3 = npsr(0, 8)
                    k.mm(ps3[:], bones_b[:], sq[:], True, True, ['bones', 'sq'], [pk3])
                    k.act(tmpc[:], ps3[:], AF.Ln, [pk3], ['tmpc'], scale=1.0 / 64, bias=lnxe[:])
                    k.act(rs[:], tmpc[:], AF.Exp, ['tmpc'], ['rs'], scale=-0.5)
                    k.tt('dve', cen[:], cen[:], rs[:], ALU.mult, ['cen', 'rs'], ['cen'])
                    k.ts('dve', cen[:], cen[:], vcol('ln_w', c), vcol('ln_b', c), ALU.mult, ALU.add, ['cen', 'vecs'], ['cen'])
                    k.tt('pool', cen[:], cen[:], bfb[:], ALU.add, ['cen', 'bfb'], ['cen'])
                    o_, okk = ybo[c % 2], ('ybo', c % 2)
                    k.tt('dve', o_[:], cen[:], gfb[:], ALU.mult, ['cen', 'gfb'], [okk])
                    k.dma('sp', YB_d[cs, t0:t0 + 512], o_[:], [okk], [])
            P.emit()
        if upto == 'C2':
            return nc

        with ExitStack() as st:
            sb = lambda n, s, d: st.enter_context(nc.sbuf_tensor(un(n), s, d))
            ya = sb("ya", [64, 8, 128], BF16)
            ybt = sb("ybt", [128, 4, 128], BF16)
            xt = sb("xt", [128, 1024], F32)
            x2 = [sb("x2_%d" % i, [128, 1024], F32) for i in range(2)]
            h2b = [sb("h2b%d" % i, [128, 1024], BF16) for i in range(2)]
            h2T = sb("h2T", [128, 8, 128], BF16)
            qTs = sb("qTs", [128, 16, 128], BF16)
            ssq = sb("ssq", [128, 1], F32)
            rstd2 = sb("rstd2", [128, 1], F32)
            junk = sb("junk", [128, 1024], BF16)
            junk2 = sb("junk2", [128, 1024], BF16)
            sc = sb("sc", [128, 16, 128], F32)
            sc2 = sb("sc2", [128, 256], F32)
            m16 = sb("m16", [128, 16, 16], F32)
            i16 = sb("i16", [128, 16, 16], U32)
            i16f = sb("i16f", [128, 16, 16], F32)
            cs_ = sb("cs", [128, 8, 256], F32)
            ts16 = sb("ts16", [128, 8, 16], F32)
            pos = sb("pos", [128, 8, 16], U32)
            pa = sb("pa", [128, 8, 16], U32)
            pb = sb("pb", [128, 8, 16], U32)
            paf = sb("paf", [128, 8, 16], F32)
            pbf = sb("pbf", [128, 8, 16], F32)
            i1s = sb("i1s", [128, 8, 16], F32)
            i2s = sb("i2s", [128, 8, 16], F32)
            eidf = sb("eidf", [128, 128], F32)
            eidx = [sb("eidx%d" % i, [128, 128], I32) for i in range(2)]
            ge = sb("ge", [128, 8, 16], F32)
            gsum = sb("gsum", [128, 8], F32)
            gate = [sb("gate%d" % i, [128, 128], F32) for i in range(2)]
            actv = sb("actv", [128, 128], F32)
            gel = sb("gel", [128, 128], F32)
            NG = 16
            gring = [sb("gring%d" % i, [128, 2048], BF16) for i in range(NG)]
            dg4 = [sb("dg4_%d" % i, [128, 4, 128], BF16) for i in range(3)]
            coef4 = [sb("coef4_%d" % i, [128, 4], BF16) for i in range(3)]
            prodr = [sb("prodr%d" % i, [128, 1024], BF16) for i in range(3)]
            ident_f = sb("ident_f", [128, 128], F32)
            k.cp("dve", ident_f[:], ident_b[:], ["ident"], ["ident_f"])
            gri = [0]
            iota16 = vecs[:, VC['iota16']:VC['iota16'] + 16]

            def top16(src_ap, srck, vals, valk, idx, idxk, scratch):
                P.op('dve', lambda e: e.max(out=vals[:, 0:8], in_=src_ap), srck, [valk])
                P.op('dve', lambda e: e.max_index(out=idx[:, 0:8], in_max=vals[:, 0:8], in_values=src_ap), srck + [valk], [idxk])
                P.op('dve', lambda e: e.match_replace(out=scratch, in_to_replace=vals[:, 0:8], in_values=src_ap, imm_value=-1e30),
                     srck + [valk], ['sc2'])
                P.op('dve', lambda e: e.max(out=vals[:, 8:16], in_=scratch), ['sc2'], [valk])
                P.op('dve', lambda e: e.max_index(out=idx[:, 8:16], in_max=vals[:, 8:16], in_values=scratch), ['sc2', valk], [idxk])

            def prologue(n):
                q = n % 2
                t0 = n * 128
                x2k, h2k = ('x2', q), ('h2', q)
                k.dma('sp', ya[:], YA_d[:, :, t0:t0 + 128].rearrange("h p t -> p h t"), (), ['ya'])
                k.dma('sp', ybt[:], YB_d[:, t0:t0 + 128].rearrange("(c p) t -> p c t", p=128), (), ['ybt'])
                k.dma('sp', xt[:], x_d[t0:t0 + 128, :], (), ['xt'])
                yield
                for half in range(2):
                    hs = slice(half * 512, (half + 1) * 512)
                    ps_t, pk = npsr(0, 6)
                    for h in range(8):
                        k.mm(ps_t[:], ya[:, h, :], w_oA[:, h, hs], h == 0, False, ['ya', 'wtsD'], [pk])
                    for c in range(4):
                        k.mm(ps_t[:], ybt[:, c, :], w_oB[:, c, hs], False, c == 3, ['ybt', 'wtsD'], [pk])
                    k.tt('dve', x2[q][:, hs], ps_t[:], xt[:, hs], ALU.add, [pk, 'xt'], [x2k])
                    yield
                k.act(junk2[:], x2[q][:], AF.Square, [x2k], ['junk2', 'ssq'], accum_out=ssq[:])
                k.act(ssq[:], ssq[:], AF.Sqrt, ['ssq'], ['ssq'], scale=1.0 / 1024, bias=EPS)
                k.recip(rstd2[:], ssq[:], ['ssq'], ['rstd2'])
                k.stt(h2b[q][:], x2[q][:], rstd2[:], gffn[:], ALU.mult, ALU.mult, [x2k, 'rstd2', 'gffn'], [h2k])
                yield
                for g2_ in range(2):
                    ps_t, pk = npsr(0, 6)
                    psb = ps_t[:].bitcast(BF16)
                    for q4 in range(4):
                        kc = g2_ * 4 + q4
                        k.tr(psb[:, q4 * 128:(q4 + 1) * 128], h2b[q][:, kc * 128:(kc + 1) * 128], ident_b[:], [h2k, 'ident'], [pk])
                    k.cp('act', h2T[:, g2_ * 4:(g2_ + 1) * 4, :], psb[:, 0:512].rearrange("p (a t) -> p a t", t=128), [pk], ['h2T'])
                    yield
                for g4 in range(4):
                    ps_t, pk = npsr(0, 6)
                    for q4 in range(4):
                        hp = g4 * 4 + q4
                        for kc in range(8):
                            k.mm(ps_t[:, q4 * 128:(q4 + 1) * 128], w_pq_b[:, kc, hp * 128:(hp + 1) * 128], h2T[:, kc, :], kc == 0, kc == 7,
                                 ['wtsD', 'h2T'], [pk])
                    k.cp('act', qTs[:, g4 * 4:(g4 + 1) * 4, :], ps_t[:].rearrange("p (a t) -> p a t", t=128), [pk], ['qTs'])
                    yield
                for g4 in range(4):
                    ps_t, pk = npsr(0, 6)
                    for q4 in range(4):
                        hp = g4 * 4 + q4
                        k.mm(ps_t[:, q4 * 128:(q4 + 1) * 128], qTs[:, hp, :], keys_b[:, hp, :], True, True, ['qTs', 'wtsD'], [pk])
                    k.cp('act', sc[:, g4 * 4:(g4 + 1) * 4, :], ps_t[:].rearrange("p (a n) -> p a n", n=128), [pk], ['sc'])
                    yield
                for hp in range(16):
                    top16(sc[:, hp, :], ['sc'], m16[:, hp, :], 'm16', i16[:, hp, :], 'i16', sc2[:, 0:128])
                    yield
                k.cp('dve', i16f[:], i16[:], ['i16'], ['i16f'])
                m4 = m16[:].rearrange("p (h two) k -> p h two k", two=2)
                i4 = i16f[:].rearrange("p (h two) k -> p h two k", two=2)
                cs4 = cs_[:].rearrange("p h (a b) -> p h a b", b=16)
                eq4 = cs4
                bc_a = lambda ap: ap.unsqueeze(3).broadcast_to([128, 8, 16, 16])
                bc_b = lambda ap: ap.unsqueeze(2).broadcast_to([128, 8, 16, 16])
                k.tt('dve', cs4, bc_a(m4[:, :, 0, :]), bc_b(m4[:, :, 1, :]), ALU.add, ['m16'], ['cs'])
                yield
                for h in range(8):
                    top16(cs_[:, h, :], ['cs'], ts16[:, h, :], 'ts16', pos[:, h, :], 'pos', sc2[:, :])
                    yield
                P.op('dve', lambda e: e.tensor_single_scalar(out=pa[:], in_=pos[:], scalar=4, op=ALU.logical_shift_right), ['pos'], ['pa'])
                P.op('dve', lambda e: e.tensor_single_scalar(out=pb[:], in_=pos[:], scalar=15, op=ALU.bitwise_and), ['pos'], ['pb'])
                k.cp('dve', paf[:], pa[:], ['pa'], ['paf'])
                k.cp('dve', pbf[:], pb[:], ['pb'], ['pbf'])
                yield
                io4 = iota16.unsqueeze(1).unsqueeze(1).broadcast_to([128, 8, 16, 16])
                for (pf, pfk, two, dst, dk) in ((paf, 'paf', 0, i1s, 'i1s'), (pbf, 'pbf', 1, i2s, 'i2s')):
                    k.tt('dve', eq4, io4, bc_a(pf[:]), ALU.is_equal, [pfk, 'vecs', 'cs'], ['cs'])
                    k.tt('dve', eq4, eq4, bc_b(i4[:, :, two, :]), ALU.mult, ['cs', 'i16f'], ['cs'])
                    P.op('dve', lambda e, dst=dst: e.tensor_reduce(out=dst[:], in_=eq4, axis=AX.X, op=ALU.add), ['cs'], [dk])
                    yield
                k.stt(eidf[:], i1s[:].rearrange("p h k -> p (h k)"), 128.0, i2s[:].rearrange("p h k -> p (h k)"),
                      ALU.mult, ALU.add, ['i1s', 'i2s'], ['eidf'])
                k.cp('dve', eidx[q][:], eidf[:], ['eidf'], [('eidx', q)])
                k.tt('dve', ge[:], ts16[:], ts16[:, :, 0:1].broadcast_to([128, 8, 16]), ALU.subtract, ['ts16'], ['ge'])
                k.act(ge[:], ge[:], AF.Exp, ['ge'], ['ge'])
                P.op('dve', lambda e: e.tensor_reduce(out=gsum[:], in_=ge[:], axis=AX.X, op=ALU.add), ['ge'], ['gsum'])
                k.recip(gsum[:], gsum[:], ['gsum'], ['gsum'])
                k.tt('dve', gate[q][:].rearrange("p (h k) -> p h k", k=16), ge[:], gsum[:].unsqueeze(2).broadcast_to([128, 8, 16]),
                     ALU.mult, ['ge', 'gsum'], [('gate', q)])
                yield

            def drain(gen, steps=None):
                if gen is None:
                    return None
                n_ = 0
                while steps is None or n_ < steps:
                    try:
                        next(gen)
                    except StopIteration:
                        return None
                    n_ += 1
                return gen

            NTL = T // 128
            drain(prologue(0))
            for n in range(NTL):
                q = n % 2
                t0 = n * 128
                gen = prologue(n + 1) if n + 1 < NTL else None
                LAG = 2
                ring_of = {}
                for sl in range(128 + LAG):
                    if sl < 128:
                        slot = sl
                        i = gri[0] % NG
                        gri[0] += 1
                        ring_of[slot] = i
                        g_, gk = gring[i], ('gring', i)
                        P.dma('pool', lambda e, g_=g_, slot=slot, q=q: e.indirect_dma_start(
                            out=g_[:], out_offset=None, in_=EUVb_d,
                            in_offset=bass.IndirectOffsetOnAxis(ap=eidx[q][:, slot:slot + 1], axis=0)), [('eidx', q)], [gk])
                        if slot % 4 == 3:
                            P.op('dve', lambda e, g_=g_, slot=slot, q=q: e.scalar_tensor_tensor(
                                out=junk2[:], in0=g_[:, 0:1024], scalar=1.0, in1=h2b[q][:], op0=ALU.mult, op1=ALU.mult,
                                accum_out=actv[:, slot:slot + 1]), [gk, ('h2', q)], [('actv', i)])
                        else:
                            pi = slot % 3
                            k.tt('dve', prodr[pi][:], g_[:, 0:1024], h2b[q][:], ALU.mult, [gk, ('h2', q)], [('prod', pi)])
                            k.act(junk[:], prodr[pi][:], AF.Copy, [('prod', pi)], [('actv', i)], accum_out=actv[:, slot:slot + 1])
                    if 1 <= sl <= 128:
                        slot = sl - 1
                        i = ring_of[slot]
                        k.act(gel[:, slot:slot + 1], actv[:, slot:slot + 1], AF.Gelu, [('actv', i)], [('gel', i)])
                    if sl >= LAG and (sl - LAG) % 4 == 3:
                        s0 = sl - LAG - 3
                        gi = (s0 // 4) % 3
                        k.tt('dve', coef4[gi][:], gel[:, s0:s0 + 4], gate[q][:, s0:s0 + 4], ALU.mult,
                             [('gel', ring_of[s_]) for s_ in range(s0, s0 + 4)] + [('gate', q)], [('coef4', gi)])
                        k.tt('dve', dg4[gi][:], ident_b[:].unsqueeze(1).broadcast_to([128, 4, 128]),
                             coef4[gi][:].unsqueeze(2).broadcast_to([128, 4, 128]), ALU.mult, ['ident', ('coef4', gi)], [('dg4', gi)])
                        for j4 in range(4):
                            slot = s0 + j4
                            i = ring_of[slot]
                            g_, gk = gring[i], ('gring', i)
                            for half in range(2):
                                k.mm(psum[6 + half][:], dg4[gi][:, j4, :], g_[:, 1024 + half * 512:1024 + (half + 1) * 512], slot == 0, slot == 127,
                                     [('dg4', gi), gk], [('ps', 6 + half)])
                    if sl >= 8:
                        gen = drain(gen, 1)
                drain(gen)
                for half in range(2):
                    hs = slice(half * 512, (half + 1) * 512)
                    k.tt('dve', x2[q][:, hs], psum[6 + half][:], x2[q][:, hs], ALU.add, [('ps', 6 + half), ('x2', q)], [('x2', q)])
                k.dma('sp', out_d[t0:t0 + 128, :], x2[q][:], [('x2', q)], [])
            P.emit()
    return nc


def prep_shared(inp):
    f = lambda n: np.ascontiguousarray(np.asarray(inp[n])[0], dtype=np.float32)
    vecs = np.zeros((128, NV), np.float32)

    def put(name, arr, ncols):
        a = np.asarray(arr, np.float32).reshape(ncols, -1)
        vecs[:a.shape[1], VC[name]:VC[name] + ncols] = a.T

    put('g_mix', f('g_mix'), 8)
    put('g_cq', f('g_cq'), 3)
    put('g_ckv', f('g_ckv'), 2)
    perm = np.concatenate([np.arange(64), np.arange(80, 96), np.arange(64, 80)])
    put('g_q', f('g_qnorm'), 1)
    put('g_qp', f('g_qnorm')[perm], 1)
    put('g_k', f('g_knorm'), 1)
    put('g_kp', f('g_knorm')[perm], 1)
    put('g_ao', f('g_attn_out'), 8)
    put('mu', f('rwkv_mu'), 14)
    for n_, src in (('w0', 'w0'), ('a0', 'a0'), ('k_k', 'k_k'), ('k_a', 'k_a'), ('r_k', 'r_k'),
                    ('ln_w', 'ln_x_w'), ('ln_b', 'ln_x_b')):
        put(n_, f(src).reshape(-1), 4)
    inv = (10000.0 ** (-np.arange(16, dtype=np.float64) / 16)).astype(np.float32)
    vecs[64:80, VC['inv']] = inv
    vecs[80:96, VC['inv']] = inv
    vecs[64:80, VC['sgn']] = -1.0
    vecs[80:96, VC['sgn']] = 1.0
    vecs[:, VC['iota16']:VC['iota16'] + 16] = np.arange(16, dtype=np.float32)[None, :]
    w_in = f('w_in')
    w_krp = np.zeros((1024, 96), np.float32)
    w_krp[:, 64:80] = w_in[:, 656:672]
    w_krp[:, 80:96] = w_in[:, 640:656]
    w_uq = f('w_uq')
    cols = np.concatenate([h * 96 + perm for h in range(8)])
    w_uqp = np.ascontiguousarray(w_uq[:, cols])
    keysT = np.ascontiguousarray(f('sub_keys').reshape(16, 128, 128).transpose(0, 2, 1))
    MU = np.zeros((128, 256), np.float32)
    ML = np.zeros((128, 128), np.float32)
    ii = np.arange(64)
    for e in range(2):
        o = e * 64
        MU[o:o + 64, o:o + 64] = (ii[:, None] < ii[None, :])
        MU[o:o + 64, 128 + o:128 + o + 64] = (ii[:, None] <= ii[None, :])
        ML[o:o + 64, o:o + 64] = (ii[:, None] > ii[None, :])
    return dict(MU=MU, ML=ML, vecs=vecs, ident=np.eye(128, dtype=np.float32), w_in=w_in, w_krp=w_krp, w_uq=w_uq, w_uqp=w_uqp,
                w_uk=f('w_uk'), w_uv=f('w_uv'), w2=f('w2'), a2=f('a2'), g2=f('g2'), w_o=f('w_o'),
                g_ffn=f('g_ffn'), w_pq=f('w_pq'), keysT=keysT, expert_uv=np.ascontiguousarray(np.concatenate([f('expert_u'), f('expert_v')], axis=1)))


def prep_core(inp, c, S):
    x = np.asarray(inp['x'], np.float32)[2 * c:2 * c + 2].reshape(2 * S, 1024)
    pos = np.asarray(inp['positions'], np.int32)[2 * c:2 * c + 2].reshape(2 * S)
    return dict(x=np.ascontiguousarray(x), xT=np.ascontiguousarray(x.T), pos=np.ascontiguousarray(pos))


def kernel(**inputs):
    S = inputs['x'].shape[1]
    B = inputs['x'].shape[0]
    ncore = B // 2
    shared = prep_shared(inputs)
    nc = build(S)
    in_maps = []
    for c in range(ncore):
        m = dict(shared)
        m.update(prep_core(inputs, c, S))
        in_maps.append(m)
    res = run_bass_kernel_spmd(nc, in_maps, core_ids=list(range(ncore)))
    out = np.stack([r["out"].reshape(2, S, 1024) for r in res.results], 0).reshape(B, S, 1024)
    return out.astype(np.float32)
```

```python
import math
import numpy as np
from contextlib import ExitStack
import concourse.bass as bass
import concourse.mybir as mybir
from concourse.bass_utils import run_bass_kernel_spmd

F32 = mybir.dt.float32
BF16 = mybir.dt.bfloat16
I32 = mybir.dt.int32
U32 = mybir.dt.uint32
AF = mybir.ActivationFunctionType
ALU = mybir.AluOpType
AX = mybir.AxisListType

EPS = 1e-6
NV = 112


class Prog:
    ENG = ['pe', 'act', 'dve', 'pool', 'sp']
    NDS = 16

    def __init__(self, nc, stack):
        self.nc = nc
        self.sem = {e: stack.enter_context(nc.semaphore("s_" + e)) for e in self.ENG}
        self.cnt = {e: 0 for e in self.ENG}
        self.dsem = {q: [stack.enter_context(nc.semaphore("d_%s%d" % (q, i))) for i in range(self.NDS)]
                     for q in ['sp', 'act', 'pool']}
        self.dcnt = {q: 0 for q in self.dsem}
        self.waited = {e: {} for e in self.ENG}
        self.ops = {e: [] for e in self.ENG}
        self.last_w = {}
        self.rd = {}
        self.all_dma = []
        self.nins = 0

    def _deps(self, eng, reads, writes, skip_engs=()):
        deps = []
        for r in reads:
            t = self.last_w.get(r)
            if t is not None:
                deps.append(t)
        for w in writes:
            t = self.last_w.get(w)
            if t is not None:
                deps.append(t)
            deps.extend(x for x in self.rd.get(w, ()) if x[3] not in skip_engs)
        wd = self.waited[eng]
        best = {}
        for (sem, val, key, teng) in deps:
            if teng == eng and eng == 'pe':
                continue
            if wd.get(key, 0) >= val:
                continue
            if best.get(key, (None, 0))[1] < val:
                best[key] = (sem, val)
        waits = []
        for key, (sem, val) in best.items():
            wd[key] = val
            waits.append((sem, val))
        return waits

    def _commit(self, tok, reads, writes):
        for w in writes:
            self.last_w[w] = tok
            self.rd[w] = []
        for r in reads:
            if r in writes:
                continue
            self.rd.setdefault(r, []).append(tok)

    def op(self, eng, fn, reads=(), writes=()):
        waits = self._deps(eng, reads, writes)
        self.cnt[eng] += 1
        tok = (self.sem[eng], self.cnt[eng], 'e_' + eng, eng)
        self.ops[eng].append((waits, fn, self.sem[eng], 1))
        self._commit(tok, reads, writes)
        self.nins += 1
        return tok

    def dma(self, q, fn, reads=(), writes=(), skip_engs=(), no_slot_wait=False):
        waits = self._deps(q, reads, writes, skip_engs)
        i = self.dcnt[q]
        self.dcnt[q] += 1
        slot = i % self.NDS
        val = 16 * (i // self.NDS + 1)
        sem = self.dsem[q][slot]
        key = 'd_%s%d' % (q, slot)
        if val > 16 and self.waited[q].get(key, 0) < val - 16 and not no_slot_wait:
            waits.append((sem, val - 16))
            self.waited[q][key] = val - 16
        tok = (sem, val, key, 'dma_' + q)
        self.ops[q].append((waits, fn, sem, 16))
        self._commit(tok, reads, writes)
        self.all_dma.append(tok)
        self.nins += 1
        return tok

    def wait_all_dma(self, eng='sp'):
        best = {}
        for (sem, val, key, _) in self.all_dma:
            if best.get(key, (None, 0))[1] < val:
                best[key] = (sem, val)
        waits = []
        for key, (sem, val) in best.items():
            if self.waited[eng].get(key, 0) < val:
                waits.append((sem, val))
                self.waited[eng][key] = val
        self.ops[eng].append((waits, None, None, 0))
        self.all_dma = []

    def emit(self):
        nc = self.nc
        ops = self.ops
        self.wait_all_dma('sp')
        with nc.Block() as block:
            def run(e, lst):
                for (waits, fn, sem, inc) in lst:
                    for (s, v) in waits:
                        e.wait_ge(s, v)
                    if fn is not None:
                        fn(e).then_inc(sem, inc)

            @block.tensor
            def _(e):
                run(e, ops['pe'])

            @block.scalar
            def _(e):
                run(e, ops['act'])

            @block.vector
            def _(e):
                run(e, ops['dve'])

            @block.gpsimd
            def _(e):
                run(e, ops['pool'])

            @block.sync
            def _(e):
                run(e, ops['sp'])
        self.ops = {e: [] for e in self.ENG}


class K:
    def __init__(self, P):
        self.P = P

    def mm(self, out, lhsT, rhs, start, stop, rd, wr):
        self.P.op('pe', lambda e: e.matmul(out, lhsT=lhsT, rhs=rhs, start=start, stop=stop), rd, wr)

    def tr(self, out, in_, ident, rd, wr):
        self.P.op('pe', lambda e: e.transpose(out, in_, ident), rd, wr)

    def act(self, out, in_, func, rd, wr, **kw):
        self.P.op('act', lambda e: e.activation(out=out, in_=in_, func=func, **kw), rd, wr)

    def tt(self, eng, out, in0, in1, op, rd, wr):
        self.P.op(eng, lambda e: e.tensor_tensor(out=out, in0=in0, in1=in1, op=op), rd, wr)

    def ts(self, eng, out, in0, s1, s2, op0, op1, rd, wr):
        if op1 is None:
            self.P.op(eng, lambda e: e.tensor_scalar(out=out, in0=in0, scalar1=s1, scalar2=None, op0=op0), rd, wr)
        else:
            self.P.op(eng, lambda e: e.tensor_scalar(out=out, in0=in0, scalar1=s1, scalar2=s2, op0=op0, op1=op1), rd, wr)

    def stt(self, out, in0, scalar, in1, op0, op1, rd, wr):
        self.P.op('dve', lambda e: e.scalar_tensor_tensor(out=out, in0=in0, scalar=scalar, in1=in1, op0=op0, op1=op1), rd, wr)

    def cp(self, eng, out, in_, rd, wr):
        if eng == 'act':
            self.P.op('act', lambda e: e.activation(out=out, in_=in_, func=AF.Copy), rd, wr)
        else:
            self.P.op(eng, lambda e: e.tensor_copy(out=out, in_=in_), rd, wr)

    def recip(self, out, in_, rd, wr):
        self.P.op('dve', lambda e: e.reciprocal(out=out, in_=in_), rd, wr)

    def memset(self, eng, ap, val, wr):
        self.P.op(eng, lambda e: e.memset(ap, val), (), wr)

    def dma(self, q, out, in_, rd, wr, slow=False):
        if slow:
            self.P.dma(q, lambda e: e.dma_start(out=out, in_=in_, allow_slow_non_contiguous=True), rd, wr)
        else:
            self.P.dma(q, lambda e: e.dma_start(out=out, in_=in_), rd, wr)


VC = {}
_c = 0
for _n, _w in [('g_mix', 8), ('g_cq', 3), ('g_ckv', 2), ('g_q', 1), ('g_qp', 1), ('g_k', 1), ('g_kp', 1),
               ('g_ao', 8), ('mu', 14), ('w0', 4), ('a0', 4), ('k_k', 4), ('k_a', 4), ('r_k', 4),
               ('ln_w', 4), ('ln_b', 4), ('inv', 1), ('sgn', 1), ('iota16', 16)]:
    VC[_n] = _c
    _c += _w
assert _c <= NV


def build(S=2048, dbg=(), upto='all'):
    T = 2 * S
    NBLK = T // 512
    BPS = S // 512
    nc = bass.Bass("TRN2", target_bir_lowering=False)
    din = lambda n, s, d: nc.dram_tensor(n, s, d, kind="ExternalInput").ap()

    def dscr(n, s, d):
        kind = "ExternalOutput" if n in dbg else "Internal"
        return nc.dram_tensor(n, s, d, kind=kind).ap()

    xT_d = din("xT", [1024, T], F32)
    x_d = din("x", [T, 1024], F32)
    pos_d = din("pos", [T], I32)
    vecs_d = din("vecs", [128, NV], F32)
    ident_d = din("ident", [128, 128], F32)
    w_in_d = din("w_in", [1024, 2464], F32)
    w_krp_d = din("w_krp", [1024, 96], F32)
    w_uq_d = din("w_uq", [384, 768], F32)
    w_uqp_d = din("w_uqp", [384, 768], F32)
    w_uk_d = din("w_uk", [256, 512], F32)
    w_uv_d = din("w_uv", [256, 512], F32)
    w2_d = din("w2", [64, 512], F32)
    a2_d = din("a2", [64, 512], F32)
    g2_d = din("g2", [128, 512], F32)
    w_o_d = din("w_o", [1024, 1024], F32)
    g_ffn_d = din("g_ffn", [1024], F32)
    w_pq_d = din("w_pq", [1024, 2048], F32)
    keysT_d = din("keysT", [16, 128, 128], F32)
    euv_d = din("expert_uv", [16384, 2048], F32)
    out_d = nc.dram_tensor("out", [T, 1024], F32, kind="ExternalOutput").ap()

    QT_d = dscr("QT", [2, 8, 96, S], BF16)
    KT_d = dscr("KT", [2, 8, 96, S], BF16)
    VA_d = dscr("VA", [T // 128, 128, 8, 128], BF16)
    KKf_d = dscr("KKf", [512, T], BF16)
    Rf_d = dscr("Rf", [512, T], BF16)
    Wf_d = dscr("Wf", [512, T], F32)
    NBt_d = dscr("NBt", [T, 512], BF16)
    KPt_d = dscr("KPt", [T, 512], BF16)
    Vt_d = dscr("Vt", [T, 512], BF16)
    Gf_d = dscr("Gf", [512, T], F32)
    Bf_d = dscr("Bf", [512, T], F32)
    Yt_d = dscr("Yt", [T, 512], BF16)
    YA_d = dscr("YA", [8, 64, T], BF16)
    YB_d = dscr("YB", [512, T], BF16)
    EUVb_d = dscr("EUVb", [16384, 2048], BF16)
    FMall_d = dscr("FMall", [4, 512, T], BF16)
    KTf_d, RTf_d, BTf_d, KKTf_d = FMall_d[0], FMall_d[1], FMall_d[2], FMall_d[3]
    TMall_d = dscr("TMall", [T, 3, 512], BF16)
    KTt_d, BHt_d, KHt_d = TMall_d[:, 0, :], TMall_d[:, 1, :], TMall_d[:, 2, :]
    WCf_d = dscr("WCf", [512, T // 64], F32)

    with ExitStack() as top:
        P = Prog(nc, top)
        k = K(P)
        psum = [top.enter_context(nc.psum_tensor("ps%d" % i, [128, 512], F32)) for i in range(8)]
        pctr = [0]

        def nps():
            i = pctr[0] % 8
            pctr[0] += 1
            return psum[i], ('ps', i)

        uid = [0]

        def un(n):
            uid[0] += 1
            return "%s_u%d" % (n, uid[0])

        csb = lambda n, s, d: top.enter_context(nc.sbuf_tensor(un(n), s, d))
        vecs = csb("vecs", [128, NV], F32)
        ident_b = csb("ident_b", [128, 128], BF16)
        ones_b = csb("ones_b", [128, 128], BF16)
        bones_b = csb("bones_b", [128, 128], BF16)
        omka = csb("omka", [128, 4], F32)
        epsc = csb("epsc", [128, 1], F32)
        lnxe = csb("lnxe", [128, 1], F32)
        k.dma('sp', vecs[:], vecs_d, (), ['vecs'])
        vcol = lambda n, j=0, lo=0, hi=128: vecs[lo:hi, VC[n] + j:VC[n] + j + 1]

        wsb = lambda n, s, d: top.enter_context(nc.sbuf_tensor(un(n), s, d))
        with ExitStack() as st:
            sb = lambda n, s, d: st.enter_context(nc.sbuf_tensor(un(n), s, d))
            stg = [sb("stg%d" % i, [128, 2464], F32) for i in range(2)]
            k.dma('sp', stg[0][:, 0:128], ident_d, (), ['stg0'])
            k.cp('dve', ident_b[:], stg[0][:, 0:128], ['stg0'], ['ident'])
            k.memset('pool', ones_b[:], 1.0, ['ones'])
            k.memset('pool', bones_b[:], 0.0, ['bones'])
            k.memset('pool', bones_b[0:64, 0:64], 1.0, ['bones'])
            k.memset('pool', bones_b[64:128, 64:128], 1.0, ['bones'])
            k.ts('dve', omka[:], vecs[:, VC['k_a']:VC['k_a'] + 4], -1.0, 1.0, ALU.mult, ALU.add, ['vecs'], ['omka'])
            k.memset('pool', epsc[:], EPS, ['epsc'])
            k.memset('pool', lnxe[:], 64e-5, ['lnxe'])
            P.emit()

        si = [1]

        def load_w(stg, dst, src, rows, cols, gname, gj):
            s_ = stg[si[0] % 2]
            key = 'stg%d' % (si[0] % 2)
            si[0] += 1
            k.dma('sp', s_[0:rows, 0:cols], src, (), [key])
            if gname is None:
                k.cp('dve', dst, s_[0:rows, 0:cols], [key], ['wts'])
            else:
                k.ts('dve', dst, s_[0:rows, 0:cols], vcol(gname, gj, 0, rows), None, ALU.mult, None,
                     [key, 'vecs'], ['wts'])

        xT_v = xT_d.rearrange("(kc p) t -> p kc t", p=128)

        def phase_inproj(mode):
            with ExitStack() as st:
                sb = lambda n, s, d: st.enter_context(nc.sbuf_tensor(un(n), s, d))
                ncol = 672 if mode == 'mla' else 1792
                c0 = 0 if mode == 'mla' else 672
                w_in_b = sb("w_in_b", [128, 8, ncol], BF16)
                if mode == 'mla':
                    w_krp_b = sb("w_krp_b", [128, 8, 96], BF16)
                    w_uq_b = sb("w_uq_b", [128, 3, 768], BF16)
                    w_uqp_b = sb("w_uqp_b", [128, 3, 768], BF16)
                    w_uk_b = sb("w_uk_b", [128, 2, 512], BF16)
                    w_uv_b = sb("w_uv_b", [128, 2, 512], BF16)
                else:
                    w2a2_b = sb("w2a2_b", [128, 512], BF16)
                    g2_b = sb("g2_b", [128, 512], BF16)
                with ExitStack() as st2:
                    stg = [st2.enter_context(nc.sbuf_tensor(un("stgw%d" % i), [128, 1792], F32)) for i in range(2)]
                    for kc in range(8):
                        load_w(stg, w_in_b[:, kc, :], w_in_d[kc * 128:(kc + 1) * 128, c0:c0 + ncol], 128, ncol, 'g_mix', kc)
                    if mode == 'mla':
                        for kc in range(8):
                            load_w(stg, w_krp_b[:, kc, :], w_krp_d[kc * 128:(kc + 1) * 128, :], 128, 96, 'g_mix', kc)
                        for kc in range(3):
                            load_w(stg, w_uq_b[:, kc, :], w_uq_d[kc * 128:(kc + 1) * 128, :], 128, 768, 'g_cq', kc)
                            load_w(stg, w_uqp_b[:, kc, :], w_uqp_d[kc * 128:(kc + 1) * 128, :], 128, 768, 'g_cq', kc)
                        for kc in range(2):
                            load_w(stg, w_uk_b[:, kc, :], w_uk_d[kc * 128:(kc + 1) * 128, :], 128, 512, 'g_ckv', kc)
                            load_w(stg, w_uv_b[:, kc, :], w_uv_d[kc * 128:(kc + 1) * 128, :], 128, 512, 'g_ckv', kc)
                    else:
                        s_ = stg[si[0] % 2]; key = 'stg%d' % (si[0] % 2); si[0] += 1
                        k.dma('sp', s_[0:64, 0:512], w2_d, (), [key])
                        k.dma('sp', s_[64:128, 0:512], a2_d, (), [key])
                        k.cp('dve', w2a2_b[:], s_[:, 0:512], [key], ['wts'])
                        load_w(stg, g2_b[:], g2_d, 128, 512, None, 0)
                    P.emit()

                xs_t = sb("xs", [128, 8, 512], F32)
                xb = sb("xb", [128, 8, 512], BF16)
                xsq = sb("xsq", [128, 8, 512], BF16)
                rstd_x = sb("rstd_x", [128, 512], F32)
                tmpa = sb("tmpa", [128, 512], F32)

                def rstd_from_ps(ps_t, pk, n, scale, dst, rows=128):
                    k.act(tmpa[0:rows, :], ps_t[0:rows, :], AF.Ln, [pk], ['tmpa'], scale=scale, bias=epsc[0:rows, :])
                    k.act(dst, tmpa[0:rows, :], AF.Exp, ['tmpa'], [n], scale=-0.5)

                def inproj(lhs_fn, m, dst, dk):
                    ps_t, pk = nps()
                    for kc in range(8):
                        k.mm(ps_t[0:m, :], lhs_fn(kc), xb[:, kc, :], kc == 0, kc == 7, ['wts', 'xb'], [pk])
                    k.tt('dve', dst, ps_t[0:m, :], rstd_x[0:m, :], ALU.mult, [pk, 'rstd_x'], [dk])

                def load_x(t0):
                    k.dma('sp', xs_t[:], xT_v[:, :, t0:t0 + 512], (), ['xs'])
                    k.cp('dve', xb[:], xs_t[:], ['xs'], ['xb'])
                    k.act(xsq[:], xs_t[:], AF.Square, ['xs'], ['xsq'])
                    ps_t, pk = nps()
                    for kc in range(8):
                        k.mm(ps_t[:], ones_b[:], xsq[:, kc, :], kc == 0, kc == 7, ['ones', 'xsq'], [pk])
                    rstd_from_ps(ps_t, pk, 'rstd_x', 1.0 / 1024, rstd_x[:])

                if mode == 'mla':
                    cq = sb("cq", [128, 3, 512], F32)
                    ckv = sb("ckv", [128, 2, 512], F32)
                    csq = sb("csq", [128, 3, 512], BF16)
                    cqn = sb("cqn", [128, 3, 512], BF16)
                    ckvn = sb("ckvn", [128, 2, 512], BF16)
                    rstd_c = sb("rstd_c", [128, 512], F32)
                    kr = sb("kr", [96, 512], F32)
                    krp = sb("krp", [96, 512], F32)
                    kcat = sb("kcat", [96, 512], F32)
                    hsq2 = [sb("hsq%d" % i, [96, 512], BF16) for i in range(2)]
                    rh2 = [sb("rh%d" % i, [96, 512], F32) for i in range(2)]
                    hn2 = [sb("hn%d" % i, [96, 512], F32) for i in range(2)]
                    hpn2 = [sb("hpn%d" % i, [96, 512], F32) for i in range(2)]
                    tmph2 = [sb("tmph%d" % i, [96, 512], F32) for i in range(2)]
                    qkT = [sb("qkT%d" % i, [96, 512], BF16) for i in range(2)]
                    vaug = sb("vaug", [128, 4, 8, 128], BF16)
                    posi = sb("posi", [96, 512], I32)
                    ang = sb("ang", [96, 512], F32)
                    rr = sb("rr", [96, 512], F32)
                    kki = sb("kki", [96, 512], I32)
                    Ct = sb("Ct", [96, 512], F32)
                    SSt = sb("SSt", [96, 512], F32)
                    k.memset('pool', vaug[:, :, :, 64:128], 1.0, ['vaug1'])
                    TWO_PI = 2.0 * math.pi
                    R = slice(64, 96)
                    for blk in range(NBLK):
                        seq = blk // BPS
                        t0 = blk * 512
                        ts_ = t0 - seq * S
                        load_x(t0)
                        k.dma('sp', posi[R, :], pos_d[t0:t0 + 512].partition_broadcast(32), (), ['posi'])
                        k.cp('dve', ang[R, :], posi[R, :], ['posi'], ['ang'])
                        k.ts('dve', ang[R, :], ang[R, :], vcol('inv', 0, 64, 96), None, ALU.mult, None, ['ang', 'vecs'], ['ang'])
                        for which, dst in (('sin', SSt), ('cos', Ct)):
                            off = 0.0 if which == 'sin' else math.pi / 2
                            k.ts('dve', rr[R, :], ang[R, :], off, 1.0 / TWO_PI, ALU.add, ALU.mult, ['ang'], ['rr'])
                            k.cp('dve', kki[R, :], rr[R, :], ['rr'], ['kki'])
                            k.cp('dve', rr[R, :], kki[R, :], ['kki'], ['rr'])
                            k.stt(dst[R, :], rr[R, :], -TWO_PI, ang[R, :], ALU.mult, ALU.add, ['rr', 'ang'], [which])
                            k.ts('dve', dst[R, :], dst[R, :], off, None, ALU.add, None, [which], [which])
                            k.ts('dve', dst[R, :], dst[R, :], math.pi, -math.pi, ALU.min, ALU.max, [which], [which])
                            k.act(dst[R, :], dst[R, :], AF.Sin, [which], [which])
                        k.ts('dve', SSt[R, :], SSt[R, :], vcol('sgn', 0, 64, 96), None, ALU.mult, None, ['sin', 'vecs'], ['sin'])

                        for c in range(3):
                            inproj(lambda kc, c=c: w_in_b[:, kc, c * 128:(c + 1) * 128], 128, cq[:, c, :], 'cq')
                        for c in range(2):
                            inproj(lambda kc, c=c: w_in_b[:, kc, 384 + c * 128:384 + (c + 1) * 128], 128, ckv[:, c, :], 'ckv')
                        inproj(lambda kc: w_in_b[:, kc, 576:672], 96, kr[:, :], 'kr')
                        inproj(lambda kc: w_krp_b[:, kc, :], 96, krp[:, :], 'krp')
                        for (src, sk_, n, dstn, dk) in ((cq, 'cq', 3, cqn, 'cqn'), (ckv, 'ckv', 2, ckvn, 'ckvn')):
                            k.act(csq[:, 0:n, :], src[:, 0:n, :], AF.Square, [sk_], ['csq'])
                            ps_t, pk = nps()
                            for c in range(n):
                                k.mm(ps_t[:], ones_b[:], csq[:, c, :], c == 0, c == n - 1, ['ones', 'csq'], [pk])
                            rstd_from_ps(ps_t, pk, 'rstd_c', 1.0 / (128 * n), rstd_c[:])
                            k.tt('dve', dstn[:, 0:n, :], src[:, 0:n, :],
                                 rstd_c[:].unsqueeze(1).broadcast_to([128, n, 512]), ALU.mult, [sk_, 'rstd_c'], [dk])
                        k.cp('act', kcat[R, :], kr[R, :], ['kr'], ['kcat_r'])
                        for h in range(8):
                            for which in ('q', 'k'):
                                hb = (h * 2 + (which == 'k')) % 2
                                hsq, rh, hn, hpn, tmph = hsq2[hb], rh2[hb], hn2[hb], hpn2[hb], tmph2[hb]
                                HSQ, RH, HN, HPN, TMPH = ('hsq', hb), ('rh', hb), ('hn', hb), ('hpn', hb), ('tmph', hb)
                                ps_t, pk = nps()
                                if which == 'q':
                                    for kc in range(3):
                                        k.mm(ps_t[0:96, :], w_uq_b[:, kc, h * 96:(h + 1) * 96], cqn[:, kc, :], kc == 0, kc == 2, ['wts', 'cqn'], [pk])
                                    ps2, pk2 = nps()
                                    for kc in range(3):
                                        k.mm(ps2[0:96, :], w_uqp_b[:, kc, h * 96:(h + 1) * 96], cqn[:, kc, :], kc == 0, kc == 2, ['wts', 'cqn'], [pk2])
                                    src_ap, srck = ps_t, [pk]
                                    k.act(hsq[:, :], ps_t[0:96, :], AF.Square, [pk], [HSQ])
                                    part_ap, partk = ps2, [pk2]
                                    gn, gpn = 'g_q', 'g_qp'
                                else:
                                    for kc in range(2):
                                        k.mm(ps_t[0:64, :], w_uk_b[:, kc, h * 64:(h + 1) * 64], ckvn[:, kc, :], kc == 0, kc == 1, ['wts', 'ckvn'], [pk])
                                    k.cp('act', kcat[0:64, :], ps_t[0:64, :], [pk], ['kcat_n'])
                                    src_ap, srck = kcat, ['kcat_n', 'kcat_r']
                                    k.act(hsq[:, :], kcat[:, :], AF.Square, srck, [HSQ])
                                    part_ap, partk = krp, ['krp']
                                    gn, gpn = 'g_k', 'g_kp'
                                ps3, pk3 = nps()
                                k.mm(ps3[0:96, :], ones_b[0:96, 0:96], hsq[:, :], True, True, ['ones', HSQ], [pk3])
                                k.act(tmph[:, :], ps3[0:96, :], AF.Ln, [pk3], [TMPH], scale=1.0 / 96, bias=epsc[0:96, :])
                                k.act(rh[:, :], tmph[:, :], AF.Exp, [TMPH], [RH], scale=-0.5)
                                oi = (h * 2 + (which == 'k')) % 2
                                ot, ok = qkT[oi], 'qkT%d' % oi
                                k.stt(ot[0:64, :], src_ap[0:64, :], vcol(gn, 0, 0, 64), rh[0:64, :], ALU.mult, ALU.mult,
                                      srck + [RH, 'vecs'], [ok])
                                k.stt(hn[R, :], src_ap[R, :], vcol(gn, 0, 64, 96), rh[R, :], ALU.mult, ALU.mult,
                                      srck + [RH, 'vecs'], [HN])
                                k.stt(hpn[R, :], part_ap[R, :], vcol(gpn, 0, 64, 96), rh[R, :], ALU.mult, ALU.mult,
                                      partk + [RH, 'vecs'], [HPN])
                                k.tt('pool', hn[R, :], hn[R, :], Ct[R, :], ALU.mult, [HN, 'cos'], [HN])
                                k.tt('dve', hpn[R, :], hpn[R, :], SSt[R, :], ALU.mult, [HPN, 'sin'], [HPN])
                                k.tt('pool', ot[R, :], hn[R, :], hpn[R, :], ALU.add, [HN, HPN], [ok])
                                dst_d = QT_d if which == 'q' else KT_d
                                k.dma('sp', dst_d[seq, h, :, ts_:ts_ + 512], ot[:, :], [ok], [])
                        for tt_ in range(4):
                            ps_t, pk = nps()
                            for kc in range(2):
                                k.mm(ps_t[:], ckvn[:, kc, tt_ * 128:(tt_ + 1) * 128], w_uv_b[:, kc, :], kc == 0, kc == 1, ['ckvn', 'wts'], [pk])
                            k.cp('act', vaug[:, tt_, :, 0:64], ps_t[:].rearrange("p (h d) -> p h d", d=64), [pk], ['vaug'])
                        k.dma('sp', VA_d[blk * 4:(blk + 1) * 4].rearrange("t p h d -> p t h d"), vaug[:], ['vaug', 'vaug1'], [])
                else:
                    pst = sb("pst", [128, 14, 513], F32)
                    carry = sb("carry", [128, 14, 1], F32)
                    dd = sb("dd", [128, 512], F32)
                    thal = sb("thal", [128, 512], BF16)
                    sgl = sb("sgl", [128, 512], BF16)
                    sig = sb("sig", [128, 512], F32)
                    dec = sb("dec", [128, 512], F32)
                    aa = sb("aa", [128, 512], F32)
                    gst = sb("gst", [128, 512], F32)
                    kkr = sb("kkr", [128, 512], F32)
                    ksq = sb("ksq", [128, 512], BF16)
                    rn = sb("rn", [128, 512], F32)
                    kkn32 = sb("kkn32", [128, 512], F32)
                    fb = [sb("fb%d" % i, [128, 512], BF16) for i in range(8)]
                    tq = sb("tq", [128, 512], F32)
                    kp32 = sb("kp32", [128, 512], F32)
                    rkk = sb("rkk", [128, 512], BF16)
                    bon = sb("bon", [128, 512], F32)
                    tokst = [sb("tokst%d" % i, [128, 4, 512], BF16) for i in range(4)]
                    lw = sb("lw", [128, 512], F32)
                    csm = sb("csm", [128, 512], F32)
                    eneg = sb("eneg", [128, 512], F32)
                    epos = sb("epos", [128, 512], F32)
                    eprev = sb("eprev", [128, 512], F32)
                    ehat = sb("ehat", [128, 512], F32)
                    wcs = sb("wcs", [128, 8], F32)
                    rmask = sb("rmask", [128, 512], F32)
                    k.memset('pool', rmask[:], 1.0, ['rmask'])
                    k.memset('pool', rmask[:].rearrange("p (c t) -> p c t", t=64)[:, :, 0:1], 0.0, ['rmask'])
                    fbi = [0]

                    def fbuf():
                        i = fbi[0] % 8
                        fbi[0] += 1
                        return fb[i], 'fb%d' % i

                    pmk = lambda c: ('pm', c)
                    for blk in range(NBLK):
                        seq = blk // BPS
                        t0 = blk * 512
                        load_x(t0)
                        if seq * BPS == blk:
                            k.memset('pool', carry[:], 0.0, ['carry'])
                        for c in range(14):
                            k.cp('pool', pst[:, c, 0:1], carry[:, c, :], ['carry'], [pmk(c)])
                            inproj(lambda kc, c=c: w_in_b[:, kc, c * 128:(c + 1) * 128], 128, pst[:, c, 1:513], pmk(c))
                            k.tt('pool', dd[:], pst[:, c, 0:512], pst[:, c, 1:513], ALU.subtract, [pmk(c)], ['dd'])
                            k.cp('pool', carry[:, c, :], pst[:, c, 512:513], [pmk(c)], ['carry'])
                            k.stt(pst[:, c, 1:513], dd[:], vcol('mu', c), pst[:, c, 1:513], ALU.mult, ALU.add,
                                  ['dd', 'vecs', pmk(c), 'carry'], [pmk(c)])
                        pm = lambda c: pst[:, c, 1:513]
                        pmr = lambda c, lo, hi: pst[lo:hi, c, 1:513]
                        k.act(thal[0:64, :], pmr(12, 0, 64), AF.Tanh, [pmk(12)], ['thal'])
                        k.cp('act', thal[64:128, :], pmr(12, 64, 128), [pmk(12)], ['thal'])
                        k.act(sgl[:], pm(13), AF.Sigmoid, [pmk(13)], ['sgl'])
                        for c in range(4):
                            cs = slice(c * 128, (c + 1) * 128)
                            ps_t, pk = nps()
                            k.mm(ps_t[:], w2a2_b[0:64, cs], thal[0:64, :], True, True, ['wts', 'thal'], [pk])
                            k.act(sig[:], ps_t[:], AF.Sigmoid, [pk, 'vecs'], ['sig'], bias=vcol('w0', c))
                            ps_t, pk = nps()
                            k.mm(ps_t[:], w2a2_b[64:128, cs], thal[64:128, :], True, True, ['wts', 'thal'], [pk])
                            k.act(aa[:], ps_t[:], AF.Sigmoid, [pk, 'vecs'], ['aa'], bias=vcol('a0', c))
                            ps_t, pk = nps()
                            k.mm(ps_t[:], g2_b[:, cs], sgl[:], True, True, ['wts', 'sgl'], [pk])
                            k.cp('act', gst[:], ps_t[:], [pk], ['gst'])
                            k.dma('sp', Gf_d[cs, t0:t0 + 512], gst[:], ['gst'], [])
                            k.ts('dve', kkr[:], pm(4 + c), vcol('k_k', c), None, ALU.mult, None, [pmk(4 + c), 'vecs'], ['kkr'])
                            k.act(ksq[:], kkr[:], AF.Square, ['kkr'], ['ksq'])
                            ps_t, pk = nps()
                            k.mm(ps_t[:], bones_b[:], ksq[:], True, True, ['bones', 'ksq'], [pk])
                            k.ts('dve', rn[:], ps_t[:], 1e-24, None, ALU.max, None, [pk], ['rn'])
                            k.act(rn[:], rn[:], AF.Ln, ['rn'], ['rn'])
                            k.act(rn[:], rn[:], AF.Exp, ['rn'], ['rn'], scale=-0.5)
                            k.tt('dve', kkn32[:], kkr[:], rn[:], ALU.mult, ['kkr', 'rn'], ['kkn32'])
                            k.ts('dve', lw[:], sig[:], -math.exp(-0.5), None, ALU.mult, None, ['sig'], ['lw'])
                            P.op('dve', lambda e: e.tensor_tensor_scan(out=csm[:], data0=rmask[:], data1=lw[:], initial=0.0,
                                                                       op0=ALU.mult, op1=ALU.add), ['lw', 'rmask'], ['csm'])
                            cs3 = csm[:].rearrange("p (c t) -> p c t", t=64)
                            k.act(eneg[:], csm[:], AF.Exp, ['csm'], ['eneg'], scale=-1.0)
                            k.act(epos[:], csm[:], AF.Exp, ['csm'], ['epos'])
                            k.tt('dve', tq[:], csm[:], lw[:], ALU.subtract, ['csm', 'lw'], ['tq'])
                            k.act(eprev[:], tq[:], AF.Exp, ['tq'], ['eprev'])
                            k.tt('dve', tq[:].rearrange("p (c t) -> p c t", t=64), cs3[:, :, 63:64].broadcast_to([128, 8, 64]), cs3,
                                 ALU.subtract, ['csm', 'eprev'], ['tq'])
                            k.act(ehat[:], tq[:], AF.Exp, ['tq'], ['ehat'])
                            k.act(wcs[:].unsqueeze(2), cs3[:, :, 63:64], AF.Exp, ['csm'], ['wcs'])
                            k.dma('sp', WCf_d[cs, blk * 8:(blk + 1) * 8], wcs[:], ['wcs'], [])
                            f_kt, fk_kt = fbuf()
                            k.tt('dve', f_kt[:], kkn32[:], eprev[:], ALU.mult, ['kkn32', 'eprev'], [fk_kt])
                            k.dma('sp', KTf_d[cs, t0:t0 + 512], f_kt[:], [fk_kt], [])
                            f_r, fk_r = fbuf()
                            k.tt('dve', f_r[:], pm(c), epos[:], ALU.mult, [pmk(c), 'epos'], [fk_r])
                            k.dma('sp', RTf_d[cs, t0:t0 + 512], f_r[:], [fk_r], [])
                            k.tt('dve', kkn32[:], kkn32[:], aa[:], ALU.mult, ['kkn32', 'aa', fk_kt], ['kkn32'])
                            f_bt, fk_bt = fbuf()
                            k.tt('dve', f_bt[:], kkn32[:], eneg[:], ALU.mult, ['kkn32', 'eneg'], [fk_bt])
                            k.dma('sp', BTf_d[cs, t0:t0 + 512], f_bt[:], [fk_bt], [])
                            f_bh, fk_bh = fbuf()
                            k.tt('dve', f_bh[:], kkn32[:], ehat[:], ALU.mult, ['kkn32', 'ehat'], [fk_bh])
                            k.ts('dve', tq[:], aa[:], vcol('k_a', c), omka[:, c:c + 1], ALU.mult, ALU.add, ['aa', 'vecs', 'omka', 'ehat'], ['tq'])
                            k.tt('dve', kp32[:], pm(4 + c), tq[:], ALU.mult, [pmk(4 + c), 'tq'], ['kp32'])
                            f_kkt, fk_kkt = fbuf()
                            k.tt('dve', f_kkt[:], kp32[:], eneg[:], ALU.mult, ['kp32', 'eneg'], [fk_kkt])
                            k.dma('sp', KKTf_d[cs, t0:t0 + 512], f_kkt[:], [fk_kkt], [])
                            f_kh, fk_kh = fbuf()
                            k.tt('dve', f_kh[:], kp32[:], ehat[:], ALU.mult, ['kp32', 'ehat'], [fk_kh])
                            f_v, fk_v = fbuf()
                            k.cp('act', f_v[:], pm(8 + c), [pmk(8 + c)], [fk_v])
                            k.stt(rkk[:], pm(c), vcol('r_k', c), kp32[:], ALU.mult, ALU.mult, [pmk(c), 'vecs', 'kp32'], ['rkk'])
                            ps_t, pk = nps()
                            k.mm(ps_t[:], bones_b[:], rkk[:], True, True, ['bones', 'rkk'], [pk])
                            k.tt('dve', bon[:], ps_t[:], pm(8 + c), ALU.mult, [pk, pmk(8 + c)], ['bon'])
                            k.dma('sp', Bf_d[cs, t0:t0 + 512], bon[:], ['bon'], [])
                            for ai, (f_t, fk_t) in enumerate(((f_kt, fk_kt), (f_bh, fk_bh), (f_kh, fk_kh), (f_v, fk_v))):
                                ps_t, pk = nps()
                                psb = ps_t[:].bitcast(BF16)
                                for tt_ in range(4):
                                    k.tr(psb[:, tt_ * 128:(tt_ + 1) * 128], f_t[:, tt_ * 128:(tt_ + 1) * 128], ident_b[:], [fk_t, 'ident'], [pk])
                                k.cp('act' if ai % 2 == 0 else 'dve', tokst[ai][:, :, cs],
                                     psb[:, 0:512].rearrange("p (t f) -> p t f", f=128), [pk], [('tokst', ai)])
                        for ai, dd_ in enumerate((KTt_d, BHt_d, KHt_d, Vt_d)):
                            k.dma('sp', dd_[t0:t0 + 512, :].rearrange("(t p) f -> p t f", p=128), tokst[ai][:], [('tokst', ai)], [])
                P.emit()

        phase_inproj('mla')
        phase_inproj('rwkv')
        if upto == 'A':
            return nc

        def npsr(lo, hi, ctr=[0]):
            i = lo + ctr[0] % (hi - lo)
            ctr[0] += 1
            return psum[i], ('ps', i)

        with ExitStack() as st:
            sb = lambda n, s, d: st.enter_context(nc.sbuf_tensor(un(n), s, d))
            NT = S // 128
            qT = [sb("qT%d" % i, [96, S], BF16) for i in range(2)]
            kT = [sb("kT%d" % i, [96, S], BF16) for i in range(2)]
            Va = [sb("Va%d" % i, [128, NT, 128], BF16) for i in range(2)]
            pt = [sb("pt%d" % i, [128, 512], BF16) for i in range(4)]
            Lm = sb("Lm", [128, 64], BF16)
            Z = sb("Z", [128, 512], BF16)
            tmpb = sb("tmpb", [64, 512], F32)
            rsb = sb("rsb", [64, 512], F32)
            yo = [sb("yo%d" % i, [64, 512], BF16) for i in range(2)]
            k.memset('pool', Lm[:], 0.0, ['Lm'])
            k.memset('pool', Lm[0:64, :], 1.0 / 64, ['Lm'])
            k.memset('pool', Lm[64:65, :], EPS, ['Lm'])
            cin = [sb("cin%d" % i, [128, 2048], F32) for i in range(3)]
            cout = [sb("cout%d" % i, [128, 2048], BF16) for i in range(3)]
            cvi = [0]

            def convert_some(nchunks):
                for _ in range(nchunks):
                    ch = cvi[0]
                    if ch >= 128 + 2:
                        return
                    cvi[0] += 1
                    if ch < 128:
                        i = ch % 3
                        k.dma('sp', cin[i][:], euv_d[ch * 128:(ch + 1) * 128, :], (), [('cin', i)])
                    if ch >= 2:
                        c2 = ch - 2
                        i = c2 % 3
                        k.cp('dve', cout[i][:], cin[i][:], [('cin', i)], [('cout', i)])
                        k.dma('sp', EUVb_d[c2 * 128:(c2 + 1) * 128, :], cout[i][:], [('cout', i)], [])

            SC = 96 ** -0.5
            it = 0
            pti = [0]
            for seq in range(2):
                for h in range(8):
                    b_ = it % 2
                    it += 1
                    k.dma('sp', qT[b_][:], QT_d[seq, h], (), [('qT', b_)])
                    k.dma('sp', kT[b_][:], KT_d[seq, h], (), [('kT', b_)])
                    k.dma('sp', Va[b_][:], VA_d[seq * NT:(seq + 1) * NT, :, h, :].rearrange("t p d -> p t d"), (), [('Va', b_)])
                    convert_some(9)
                    for qb in range(S // 512):
                        po, pok = psum[6 + qb % 2], ('ps', 6 + qb % 2)
                        nkt = 4 * (qb + 1)
                        pend = []

                        def qk(kt):
                            j = kt - 4 * qb
                            c0 = max(0, 128 * j)
                            ps_t, pk = npsr(0, 6)
                            k.mm(ps_t[:, c0:512], kT[b_][:, kt * 128:(kt + 1) * 128], qT[b_][:, qb * 512 + c0:(qb + 1) * 512],
                                 True, True, [('kT', b_), ('qT', b_)], [pk])
                            i_ = pti[0] % 4
                            pti[0] += 1
                            p_, ptk = pt[i_], ('pt', i_)
                            k.act(p_[:, c0:512], ps_t[:, c0:512], AF.Exp, [pk], [ptk], scale=SC)
                            if j >= 0:
                                k.memset('pool', p_[64:128, c0:c0 + 64], 0.0, [ptk])
                            return (kt, c0, p_, ptk)

                        def pv(item):
                            kt, c0, p_, ptk = item
                            k.mm(po[:, c0:512], Va[b_][:, kt, :], p_[:, c0:512], kt == 0, kt == nkt - 1, [('Va', b_), ptk], [pok])

                        for kt in range(nkt):
                            pend.append(qk(kt))
                            if len(pend) > 2:
                                pv(pend.pop(0))
                        while pend:
                            pv(pend.pop(0))
                        k.act(Z[:], po[:], AF.Square, [pok], ['Z'])
                        ps_t, pk = npsr(0, 6)
                        k.mm(ps_t[0:64, :], Lm[:], Z[:], True, True, ['Lm', 'Z'], [pk])
                        k.act(tmpb[:], ps_t[0:64, :], AF.Ln, [pk], ['tmpb'])
                        k.act(rsb[:], tmpb[:], AF.Exp, ['tmpb'], ['rsb'], scale=-0.5)
                        y_, yk = yo[qb % 2], ('yo', qb % 2)
                        k.stt(y_[:], po[0:64, :], vecs[0:64, VC['g_ao'] + h:VC['g_ao'] + h + 1], rsb[:], ALU.mult, ALU.mult,
                              [pok, 'rsb', 'vecs'], [yk])
                        k.dma('sp', YA_d[h, :, seq * S + qb * 512:seq * S + (qb + 1) * 512], y_[:], [yk], [])
            P.emit()
        if upto == 'B':
            return nc

        w_oA = csb("w_oA", [64, 8, 1024], BF16)
        w_oB = csb("w_oB", [128, 4, 1024], BF16)
        w_pq_b = csb("w_pq_b", [128, 8, 2048], BF16)
        keys_b = csb("keys_b", [128, 16, 128], BF16)
        gffn = csb("gffn", [128, 1024], F32)
        MU_d = nc.dram_tensor("MU", [128, 256], F32, kind="ExternalInput").ap()
        ML_d = nc.dram_tensor("ML", [128, 128], F32, kind="ExternalInput").ap()
        with ExitStack() as st:
            sb = lambda n, s, d: st.enter_context(nc.sbuf_tensor(un(n), s, d))
            NCH = S // 64
            MU = sb("MUs", [128, 256], F32)
            ML = sb("MLs", [128, 128], F32)
            ident_f = sb("ident_fc", [128, 128], F32)
            k.dma('sp', MU[:], MU_d, (), ['MU'])
            k.dma('sp', ML[:], ML_d, (), ['ML'])
            k.cp('dve', ident_f[:], ident_b[:], ['ident'], ['ident_f'])
            FM = [sb("FM%d" % i, [128, 2, 4, 4, 128], BF16) for i in range(2)]
            TM = [sb("TM%d" % i, [128, 2, 3, 4, 128], BF16) for i in range(2)]
            TV = [sb("TV%d" % i, [128, 8, 64], BF16) for i in range(2)]
            WCt = sb("WCt", [128, 8, NCH], F32)
            for i in range(2):
                k.memset('pool', FM[i][:], 0.0, [('FM', i)])
                k.memset('pool', TM[i][:], 0.0, [('TM', i)])
            Sst = sb("Sst", [128, 8, 64], F32)
            Sb = sb("Sb", [128, 8, 64], BF16)
            k.memset('dve', Sst[:], 0.0, ['Sst'])
            k.memset('dve', Sb[:], 0.0, ['Sb'])
            stgd = [sb("stgd%d" % i, [128, 2048], F32) for i in range(2)]
            sdi = [0]

            def load_wd(dst, src, rows, cols, view=None):
                s_ = stgd[sdi[0] % 2]
                key = ('stgd', sdi[0] % 2)
                sdi[0] += 1
                sv = s_[0:rows, 0:cols] if view is None else view(s_)
                k.dma('sp', sv, src, (), [key])
                k.cp('act' if sdi[0] % 2 else 'dve', dst, sv, [key], ['wtsD'])

            for h in range(8):
                load_wd(w_oA[:, h, :], w_o_d[h * 64:(h + 1) * 64, :], 64, 1024)
            for c in range(4):
                load_wd(w_oB[:, c, :], w_o_d[512 + c * 128:512 + (c + 1) * 128, :], 128, 1024)
            for kc in range(8):
                load_wd(w_pq_b[:, kc, :], w_pq_d[kc * 128:(kc + 1) * 128, :], 128, 2048)
            for g4 in range(4):
                load_wd(keys_b[:, g4 * 4:(g4 + 1) * 4, :], keysT_d[g4 * 4:(g4 + 1) * 4].rearrange("a d n -> d a n"), 128, 512,
                        view=lambda s_: s_[:, 0:512].rearrange("p (a n) -> p a n", n=128))
            k.dma('sp', gffn[:], g_ffn_d.partition_broadcast(128), (), ['gffn'])
            ATB = sb("ATB", [128, 8, 256], BF16)
            AKB = sb("AKB", [128, 8, 256], BF16)
            AL = sb("AL", [128, 8, 128], BF16)
            MLv = [sb("MLv%d" % i, [128, 8, 256], BF16) for i in range(5)]
            AKV = sb("AKV", [128, 8, 64], BF16)
            X = [sb("X%d" % i, [128, 8, 192], BF16) for i in range(2)]
            GH = sb("GH", [128, 8, 192], BF16)
            PhiT = sb("PhiT", [128, 8, 128], BF16)
            OmT = sb("OmT", [128, 8, 128], BF16)
            PsY = sb("PsY", [128, 8, 128], F32)
            Yacc = [sb("Yacc%d" % i, [128, 8, 64], BF16) for i in range(2)]
            fm_src = (KTf_d, RTf_d, BTf_d, KKTf_d)
            tm_src = (KTt_d, BHt_d, KHt_d)
            dq = ['sp', 'pool']
            dqi = [0]

            def qn():
                dqi[0] += 1
                return dq[dqi[0] % 2]

            for b in range(2):
                for p in range(4):
                    k.dma(qn(), WCt[:, b * 4 + p, :], WCf_d[p * 128:(p + 1) * 128, b * NCH:(b + 1) * NCH], (), ['WCt'])

            def load_chunk(ci):
                bi = ci % 2
                for b in range(2):
                    tok = slice(b * S + ci * 64, b * S + (ci + 1) * 64)
                    for e in range(2):
                        er = slice(e * 64, (e + 1) * 64)
                        k.dma(qn(), FM[bi][er, b, :, :, e * 64:(e + 1) * 64].rearrange("q a p t -> q (a p) t"),
                              FMall_d[:, :, tok].rearrange("a (p q) t -> q (a p) t", q=128)[er], (), [('FM', bi)])
                        k.dma(qn(), TM[bi][er, b, :, :, e * 64:(e + 1) * 64].rearrange("t a p j -> t (a p) j"),
                              TMall_d[tok, :, :].rearrange("t a (p q) -> t (a p) q", q=128)[:, :, er], (), [('TM', bi)])
                        k.dma(qn(), TV[bi][er, b * 4:(b + 1) * 4, :],
                              Vt_d[tok, :].rearrange("t (p q) -> t p q", q=128)[:, :, er], (), [('TV', bi)])

            G8 = range(8)
            evi = [0]

            for ci in range(NCH):
                bi = ci % 2
                load_chunk(ci)
                fm, tm, tv = FM[bi], TM[bi], TV[bi]
                FMV = lambda g, a, fm=fm: fm[:, g // 4, a, g % 4, :]
                TMV = lambda g, a, tm=tm: tm[:, g // 4, a, g % 4, :]
                FMKR = lambda g, fm=fm: fm[:, g // 4, 0:2, g % 4, :]
                fk, tk_, vk = ('FM', bi), ('TM', bi), ('TV', bi)
                bank = lambda g: (psum[g], ('ps', g))
                for g in G8:
                    ps_t, pk = bank(g)
                    k.mm(ps_t[:, 0:256].rearrange("p (a t) -> p a t", a=2), FMV(g, 2), FMKR(g), True, True, [fk], [pk])
                    k.tt('dve', ATB[:, g, :], ps_t[:, 0:256], MU[:], ALU.mult, [pk, 'MU'], [('ATB', g)])
                for g in G8:
                    ps_t, pk = bank(g)
                    k.mm(ps_t[:, 0:256].rearrange("p (a t) -> p a t", a=2), FMV(g, 3), FMKR(g), True, True, [fk], [pk])
                    k.tt('dve', AKB[:, g, :], ps_t[:, 0:256], MU[:], ALU.mult, [pk, 'MU'], [('AKB', g)])
                for g in G8:
                    ps_t, pk = bank(g)
                    k.mm(ps_t[:, 0:128], FMV(g, 0), FMV(g, 2), True, True, [fk], [pk])
                    k.tt('dve', AL[:, g, :], ps_t[:, 0:128], ML[:], ALU.mult, [pk, 'ML'], [('AL', g)])
                for g in G8:
                    ps_t, pk = bank(g)
                    k.mm(ps_t[:, 0:64], AKB[:, g, 0:128], tv[:, g, :], True, True, [('AKB', g), vk], [pk])
                    k.cp('act', AKV[:, g, :], ps_t[:, 0:64], [pk], [('AKV', g)])
                for g in G8:
                    ps_t, pk = bank(g)
                    k.mm(ps_t[:, 0:128], ATB[:, g, 0:128], TMV(g, 0), True, True, [('ATB', g), tk_], [pk])
                    k.mm(ps_t[:, 128:192], ATB[:, g, 0:128], AKV[:, g, :], True, True, [('ATB', g), ('AKV', g)], [pk])
                    k.tt('dve', X[0][:, g, 0:128], TMV(g, 0), ps_t[:, 0:128], ALU.subtract, [pk, tk_], [('X0', g)])
                    k.tt('dve', X[0][:, g, 128:192], AKV[:, g, :], ps_t[:, 128:192], ALU.subtract, [pk, ('AKV', g)], [('X0', g)])
                xi = 0
                for lvl in range(5):
                    last = (lvl == 4)
                    for g in G8:
                        ps_t, pk = bank(g)
                        if lvl == 0:
                            Mk, Lk, mk_ = ATB[:, g, 0:128], AL[:, g, :], [('ATB', g), ('AL', g)]
                        else:
                            Mk, Lk, mk_ = MLv[lvl - 1][:, g, 0:128], MLv[lvl - 1][:, g, 128:256], [('MLv', lvl - 1, g)]
                        k.mm(ps_t[:, 0:128], Lk, Mk, True, True, mk_, [pk])
                        if not last:
                            k.mm(ps_t[:, 128:256], Mk, Lk, True, True, mk_, [pk])
                            k.cp('act', MLv[lvl][:, g, :], ps_t[:, 0:256], [pk], [('MLv', lvl, g)])
                        else:
                            k.cp('act', MLv[lvl][:, g, 0:128], ps_t[:, 0:128], [pk], [('MLv', lvl, g)])
                    for g in G8:
                        ps_t, pk = bank(g)
                        xs_, xd_ = X[xi], X[1 - xi]
                        k.mm(ps_t[:, 0:192], MLv[lvl][:, g, 0:128], xs_[:, g, :], True, True, [('MLv', lvl, g), ('X%d' % xi, g)], [pk])
                        if not last:
                            k.tt('dve', xd_[:, g, :], xs_[:, g, :], ps_t[:, 0:192], ALU.add, [pk, ('X%d' % xi, g)], [('X%d' % (1 - xi), g)])
                        else:
                            k.stt(GH[:, g, :], ps_t[:, 0:192], -1.0, xs_[:, g, :], ALU.mult, ALU.subtract, [pk, ('X%d' % xi, g)], [('GH', g)])
                    xi = 1 - xi
                for g in G8:
                    ps_t, pk = bank(g)
                    k.mm(ps_t[:, 0:128], GH[:, g, 0:128], TMV(g, 1), True, True, [('GH', g), tk_], [pk])
                    k.mm(ps_t[:, 128:256], GH[:, g, 0:128], ATB[:, g, 128:256], True, True, [('GH', g), ('ATB', g)], [pk])
                    k.stt(PhiT[:, g, :], ident_f[:], WCt[:, g, ci:ci + 1], ps_t[:, 0:128], ALU.mult, ALU.add, [pk, 'ident_f', 'WCt'], [('PhiT', g)])
                    k.tt('dve', OmT[:, g, :], ps_t[:, 128:256], FMV(g, 1), ALU.add, [pk, fk], [('OmT', g)])
                for g in G8:
                    ps_t, pk = bank(g)
                    k.mm(ps_t[:, 0:64], TMV(g, 1), GH[:, g, 128:192], True, False, [tk_, ('GH', g)], [pk])
                    k.mm(ps_t[:, 0:64], TMV(g, 2), tv[:, g, :], False, True, [tk_, vk], [pk])
                    k.mm(ps_t[:, 64:128], ATB[:, g, 128:256], GH[:, g, 128:192], True, False, [('ATB', g), ('GH', g)], [pk])
                    k.mm(ps_t[:, 64:128], AKB[:, g, 128:256], tv[:, g, :], False, True, [('AKB', g), vk], [pk])
                    k.cp('act', PsY[:, g, :], ps_t[:, 0:128], [pk], [('PsY', g)])
                ya = Yacc[ci % 2]
                yk = ('Yacc', ci % 2)
                for g in G8:
                    ps_t, pk = bank(g)
                    k.mm(ps_t[:, 0:64], OmT[:, g, :], Sb[:, g, :], True, True, [('OmT', g), ('Sb', g)], [pk])
                    k.mm(ps_t[:, 64:128], PhiT[:, g, :], Sb[:, g, :], True, True, [('PhiT', g), ('Sb', g)], [pk])
                    k.tt('dve', ya[:, g, :], ps_t[:, 0:64], PsY[:, g, 64:128], ALU.add, [pk, ('PsY', g)], [yk])
                    k.tt('dve', Sst[:, g, :], ps_t[:, 64:128], PsY[:, g, 0:64], ALU.add, [pk, ('PsY', g)], [('Sst', g)])
                    k.cp('act', Sb[:, g, :], Sst[:, g, :], [('Sst', g)], [('Sb', g)])
                for b in range(2):
                    tok = slice(b * S + ci * 64, b * S + (ci + 1) * 64)
                    for e in range(2):
                        er = slice(e * 64, (e + 1) * 64)
                        k.dma(qn(), Yt_d[tok, :].rearrange("t (p q) -> t p q", q=128)[:, :, er], ya[er, b * 4:(b + 1) * 4, :], [yk], [])
            P.emit()
        if upto == 'C':
            return nc

        with ExitStack() as st:
            sb = lambda n, s, d: st.enter_context(nc.sbuf_tensor(un(n), s, d))
            ytok = sb("ytok", [128, 4, 512], BF16)
            yb = sb("yb", [128, 512], BF16)
            cen = sb("cen", [128, 512], F32)
            sq = sb("sq", [128, 512], BF16)
            tmpc = sb("tmpc", [128, 512], F32)
            rs = sb("rs", [128, 512], F32)
            bfb = sb("bfb", [128, 512], F32)
            gfb = sb("gfb", [128, 512], F32)
            ybo = [sb("ybo%d" % i, [128, 512], BF16) for i in range(2)]
            for blk in range(NBLK):
                t0 = blk * 512
                k.dma('sp', ytok[:], Yt_d[t0:t0 + 512, :].rearrange("(t p) f -> p t f", p=128), (), ['ytok'])
                for c in range(4):
                    cs = slice(c * 128, (c + 1) * 128)
                    ps_t, pk = npsr(0, 8)
                    psb = ps_t[:].bitcast(BF16)
                    for tt_ in range(4):
                        k.tr(psb[:, tt_ * 128:(tt_ + 1) * 128], ytok[:, tt_, cs], ident_b[:], ['ytok', 'ident'], [pk])
                    k.cp('act', yb[:], psb[:, 0:512], [pk], ['yb'])
                    k.dma('sp', bfb[:], Bf_d[cs, t0:t0 + 512], (), ['bfb'])
                    k.dma('sp', gfb[:], Gf_d[cs, t0:t0 + 512], (), ['gfb'])
                    ps2, pk2 = npsr(0, 8)
                    k.mm(ps2[:], bones_b[:], yb[:], True, True, ['bones', 'yb'], [pk2])
                    k.stt(cen[:], ps2[:], -1.0 / 64, yb[:], ALU.mult, ALU.add, [pk2, 'yb'], ['cen'])
                    k.act(sq[:], cen[:], AF.Square, ['cen'], ['sq'])
                    ps3, pk3 = npsr(0, 8)
                    k.mm(ps3[:], bones_b[:], sq[:], True, True, ['bones', 'sq'], [pk3])
                    k.act(tmpc[:], ps3[:], AF.Ln, [pk3], ['tmpc'], scale=1.0 / 64, bias=lnxe[:])
                    k.act(rs[:], tmpc[:], AF.Exp, ['tmpc'], ['rs'], scale=-0.5)
                    k.tt('dve', cen[:], cen[:], rs[:], ALU.mult, ['cen', 'rs'], ['cen'])
                    k.ts('dve', cen[:], cen[:], vcol('ln_w', c), vcol('ln_b', c), ALU.mult, ALU.add, ['cen', 'vecs'], ['cen'])
                    k.tt('pool', cen[:], cen[:], bfb[:], ALU.add, ['cen', 'bfb'], ['cen'])
                    o_, okk = ybo[c % 2], ('ybo', c % 2)
                    k.tt('dve', o_[:], cen[:], gfb[:], ALU.mult, ['cen', 'gfb'], [okk])
                    k.dma('sp', YB_d[cs, t0:t0 + 512], o_[:], [okk], [])
            P.emit()
        if upto == 'C2':
            return nc

        with ExitStack() as st:
            sb = lambda n, s, d: st.enter_context(nc.sbuf_tensor(un(n), s, d))
            ya = sb("ya", [64, 8, 128], BF16)
            ybt = sb("ybt", [128, 4, 128], BF16)
            xt = sb("xt", [128, 1024], F32)
            x2 = [sb("x2_%d" % i, [128, 1024], F32) for i in range(2)]
            h2b = [sb("h2b%d" % i, [128, 1024], BF16) for i in range(2)]
            h2T = sb("h2T", [128, 8, 128], BF16)
            qTs = sb("qTs", [128, 16, 128], BF16)
            ssq = sb("ssq", [128, 1], F32)
            rstd2 = sb("rstd2", [128, 1], F32)
            junk = sb("junk", [128, 1024], BF16)
            junk2 = sb("junk2", [128, 1024], BF16)
            sc = sb("sc", [128, 16, 128], F32)
            sc2 = sb("sc2", [128, 256], F32)
            m16 = sb("m16", [128, 16, 16], F32)
            i16 = sb("i16", [128, 16, 16], U32)
            i16f = sb("i16f", [128, 16, 16], F32)
            cs_ = sb("cs", [128, 8, 256], F32)
            ts16 = sb("ts16", [128, 8, 16], F32)
            pos = sb("pos", [128, 8, 16], U32)
            pa = sb("pa", [128, 8, 16], U32)
            pb = sb("pb", [128, 8, 16], U32)
            paf = sb("paf", [128, 8, 16], F32)
            pbf = sb("pbf", [128, 8, 16], F32)
            i1s = sb("i1s", [128, 8, 16], F32)
            i2s = sb("i2s", [128, 8, 16], F32)
            eidf = sb("eidf", [128, 128], F32)
            eidx = [sb("eidx%d" % i, [128, 128], I32) for i in range(2)]
            ge = sb("ge", [128, 8, 16], F32)
            gsum = sb("gsum", [128, 8], F32)
            gate = [sb("gate%d" % i, [128, 128], F32) for i in range(2)]
            actv = sb("actv", [128, 128], F32)
            gel = sb("gel", [128, 128], F32)
            NG = 12
            gring = [sb("gring%d" % i, [128, 2048], BF16) for i in range(NG)]
            dgr = [sb("dgr%d" % i, [128, 128], BF16) for i in range(NG)]
            prodr = [sb("prodr%d" % i, [128, 1024], BF16) for i in range(3)]
            ident_f = sb("ident_f", [128, 128], F32)
            k.cp("dve", ident_f[:], ident_b[:], ["ident"], ["ident_f"])
            gri = [0]
            iota16 = vecs[:, VC['iota16']:VC['iota16'] + 16]

            def top16(src_ap, srck, vals, valk, idx, idxk, scratch):
                P.op('dve', lambda e: e.max(out=vals[:, 0:8], in_=src_ap), srck, [valk])
                P.op('dve', lambda e: e.max_index(out=idx[:, 0:8], in_max=vals[:, 0:8], in_values=src_ap), srck + [valk], [idxk])
                P.op('dve', lambda e: e.match_replace(out=scratch, in_to_replace=vals[:, 0:8], in_values=src_ap, imm_value=-1e30),
                     srck + [valk], ['sc2'])
                P.op('dve', lambda e: e.max(out=vals[:, 8:16], in_=scratch), ['sc2'], [valk])
                P.op('dve', lambda e: e.max_index(out=idx[:, 8:16], in_max=vals[:, 8:16], in_values=scratch), ['sc2', valk], [idxk])

            def prologue(n):
                q = n % 2
                t0 = n * 128
                x2k, h2k = ('x2', q), ('h2', q)
                k.dma('sp', ya[:], YA_d[:, :, t0:t0 + 128].rearrange("h p t -> p h t"), (), ['ya'])
                k.dma('sp', ybt[:], YB_d[:, t0:t0 + 128].rearrange("(c p) t -> p c t", p=128), (), ['ybt'])
                k.dma('sp', xt[:], x_d[t0:t0 + 128, :], (), ['xt'])
                yield
                for half in range(2):
                    hs = slice(half * 512, (half + 1) * 512)
                    ps_t, pk = npsr(0, 6)
                    for h in range(8):
                        k.mm(ps_t[:], ya[:, h, :], w_oA[:, h, hs], h == 0, False, ['ya', 'wtsD'], [pk])
                    for c in range(4):
                        k.mm(ps_t[:], ybt[:, c, :], w_oB[:, c, hs], False, c == 3, ['ybt', 'wtsD'], [pk])
                    k.tt('dve', x2[q][:, hs], ps_t[:], xt[:, hs], ALU.add, [pk, 'xt'], [x2k])
                    yield
                k.act(junk2[:], x2[q][:], AF.Square, [x2k], ['junk2', 'ssq'], accum_out=ssq[:])
                k.act(ssq[:], ssq[:], AF.Sqrt, ['ssq'], ['ssq'], scale=1.0 / 1024, bias=EPS)
                k.recip(rstd2[:], ssq[:], ['ssq'], ['rstd2'])
                k.stt(h2b[q][:], x2[q][:], rstd2[:], gffn[:], ALU.mult, ALU.mult, [x2k, 'rstd2', 'gffn'], [h2k])
                yield
                for g2_ in range(2):
                    ps_t, pk = npsr(0, 6)
                    psb = ps_t[:].bitcast(BF16)
                    for q4 in range(4):
                        kc = g2_ * 4 + q4
                        k.tr(psb[:, q4 * 128:(q4 + 1) * 128], h2b[q][:, kc * 128:(kc + 1) * 128], ident_b[:], [h2k, 'ident'], [pk])
                    k.cp('act', h2T[:, g2_ * 4:(g2_ + 1) * 4, :], psb[:, 0:512].rearrange("p (a t) -> p a t", t=128), [pk], ['h2T'])
                    yield
                for g4 in range(4):
                    ps_t, pk = npsr(0, 6)
                    for q4 in range(4):
                        hp = g4 * 4 + q4
                        for kc in range(8):
                            k.mm(ps_t[:, q4 * 128:(q4 + 1) * 128], w_pq_b[:, kc, hp * 128:(hp + 1) * 128], h2T[:, kc, :], kc == 0, kc == 7,
                                 ['wtsD', 'h2T'], [pk])
                    k.cp('act', qTs[:, g4 * 4:(g4 + 1) * 4, :], ps_t[:].rearrange("p (a t) -> p a t", t=128), [pk], ['qTs'])
                    yield
                for g4 in range(4):
                    ps_t, pk = npsr(0, 6)
                    for q4 in range(4):
                        hp = g4 * 4 + q4
                        k.mm(ps_t[:, q4 * 128:(q4 + 1) * 128], qTs[:, hp, :], keys_b[:, hp, :], True, True, ['qTs', 'wtsD'], [pk])
                    k.cp('act', sc[:, g4 * 4:(g4 + 1) * 4, :], ps_t[:].rearrange("p (a n) -> p a n", n=128), [pk], ['sc'])
                    yield
                for hp in range(16):
                    top16(sc[:, hp, :], ['sc'], m16[:, hp, :], 'm16', i16[:, hp, :], 'i16', sc2[:, 0:128])
                    yield
                k.cp('dve', i16f[:], i16[:], ['i16'], ['i16f'])
                m4 = m16[:].rearrange("p (h two) k -> p h two k", two=2)
                i4 = i16f[:].rearrange("p (h two) k -> p h two k", two=2)
                cs4 = cs_[:].rearrange("p h (a b) -> p h a b", b=16)
                eq4 = cs4
                bc_a = lambda ap: ap.unsqueeze(3).broadcast_to([128, 8, 16, 16])
                bc_b = lambda ap: ap.unsqueeze(2).broadcast_to([128, 8, 16, 16])
                k.tt('dve', cs4, bc_a(m4[:, :, 0, :]), bc_b(m4[:, :, 1, :]), ALU.add, ['m16'], ['cs'])
                yield
                for h in range(8):
                    top16(cs_[:, h, :], ['cs'], ts16[:, h, :], 'ts16', pos[:, h, :], 'pos', sc2[:, :])
                    yield
                P.op('dve', lambda e: e.tensor_single_scalar(out=pa[:], in_=pos[:], scalar=4, op=ALU.logical_shift_right), ['pos'], ['pa'])
                P.op('dve', lambda e: e.tensor_single_scalar(out=pb[:], in_=pos[:], scalar=15, op=ALU.bitwise_and), ['pos'], ['pb'])
                k.cp('dve', paf[:], pa[:], ['pa'], ['paf'])
                k.cp('dve', pbf[:], pb[:], ['pb'], ['pbf'])
                yield
                io4 = iota16.unsqueeze(1).unsqueeze(1).broadcast_to([128, 8, 16, 16])
                for (pf, pfk, two, dst, dk) in ((paf, 'paf', 0, i1s, 'i1s'), (pbf, 'pbf', 1, i2s, 'i2s')):
                    k.tt('dve', eq4, io4, bc_a(pf[:]), ALU.is_equal, [pfk, 'vecs', 'cs'], ['cs'])
                    k.tt('dve', eq4, eq4, bc_b(i4[:, :, two, :]), ALU.mult, ['cs', 'i16f'], ['cs'])
                    P.op('dve', lambda e, dst=dst: e.tensor_reduce(out=dst[:], in_=eq4, axis=AX.X, op=ALU.add), ['cs'], [dk])
                    yield
                k.stt(eidf[:], i1s[:].rearrange("p h k -> p (h k)"), 128.0, i2s[:].rearrange("p h k -> p (h k)"),
                      ALU.mult, ALU.add, ['i1s', 'i2s'], ['eidf'])
                k.cp('dve', eidx[q][:], eidf[:], ['eidf'], [('eidx', q)])
                k.tt('dve', ge[:], ts16[:], ts16[:, :, 0:1].broadcast_to([128, 8, 16]), ALU.subtract, ['ts16'], ['ge'])
                k.act(ge[:], ge[:], AF.Exp, ['ge'], ['ge'])
                P.op('dve', lambda e: e.tensor_reduce(out=gsum[:], in_=ge[:], axis=AX.X, op=ALU.add), ['ge'], ['gsum'])
                k.recip(gsum[:], gsum[:], ['gsum'], ['gsum'])
                k.tt('dve', gate[q][:].rearrange("p (h k) -> p h k", k=16), ge[:], gsum[:].unsqueeze(2).broadcast_to([128, 8, 16]),
                     ALU.mult, ['ge', 'gsum'], [('gate', q)])
                yield

            def drain(gen, steps=None):
                if gen is None:
                    return None
                n_ = 0
                while steps is None or n_ < steps:
                    try:
                        next(gen)
                    except StopIteration:
                        return None
                    n_ += 1
                return gen

            NTL = T // 128
            drain(prologue(0))
            for n in range(NTL):
                q = n % 2
                t0 = n * 128
                gen = prologue(n + 1) if n + 1 < NTL else None
                LAG = 3
                ring_of = {}
                for sl in range(128 + LAG):
                    if sl < 128:
                        slot = sl
                        i = gri[0] % NG
                        gri[0] += 1
                        ring_of[slot] = i
                        g_, gk = gring[i], ('gring', i)
                        P.dma('pool', lambda e, g_=g_, slot=slot, q=q: e.indirect_dma_start(
                            out=g_[:], out_offset=None, in_=EUVb_d,
                            in_offset=bass.IndirectOffsetOnAxis(ap=eidx[q][:, slot:slot + 1], axis=0)), [('eidx', q)], [gk],
                            skip_engs=('dve',), no_slot_wait=True)
                        if slot % 4 == 3:
                            P.op('dve', lambda e, g_=g_, slot=slot, q=q: e.scalar_tensor_tensor(
                                out=junk2[:], in0=g_[:, 0:1024], scalar=1.0, in1=h2b[q][:], op0=ALU.mult, op1=ALU.mult,
                                accum_out=actv[:, slot:slot + 1]), [gk, ('h2', q)], [('actv', i)])
                        else:
                            pi = slot % 3
                            k.tt('dve', prodr[pi][:], g_[:, 0:1024], h2b[q][:], ALU.mult, [gk, ('h2', q)], [('prod', pi)])
                            k.act(junk[:], prodr[pi][:], AF.Copy, [('prod', pi)], [('actv', i)], accum_out=actv[:, slot:slot + 1])
                    if 1 <= sl <= 128:
                        slot = sl - 1
                        i = ring_of[slot]
                        k.act(gel[:, slot:slot + 1], actv[:, slot:slot + 1], AF.Gelu, [('actv', i)], [('gel', i)])
                    if sl >= LAG:
                        slot = sl - LAG
                        i = ring_of[slot]
                        g_, gk = gring[i], ('gring', i)
                        k.ts('dve', dgr[i][:], ident_f[:], gel[:, slot:slot + 1], gate[q][:, slot:slot + 1], ALU.mult, ALU.mult,
                             ['ident_f', ('gel', i), ('gate', q)], [('dgr', i)])
                        for half in range(2):
                            k.mm(psum[6 + half][:], dgr[i][:], g_[:, 1024 + half * 512:1024 + (half + 1) * 512], slot == 0, slot == 127,
                                 [('dgr', i), gk], [('ps', 6 + half)])
                    if sl >= 8:
                        gen = drain(gen, 1)
                drain(gen)
                for half in range(2):
                    hs = slice(half * 512, (half + 1) * 512)
                    k.tt('dve', x2[q][:, hs], psum[6 + half][:], x2[q][:, hs], ALU.add, [('ps', 6 + half), ('x2', q)], [('x2', q)])
                k.dma('sp', out_d[t0:t0 + 128, :], x2[q][:], [('x2', q)], [])
            P.emit()
    return nc


def prep_shared(inp):
    f = lambda n: np.ascontiguousarray(np.asarray(inp[n])[0], dtype=np.float32)
    vecs = np.zeros((128, NV), np.float32)

    def put(name, arr, ncols):
        a = np.asarray(arr, np.float32).reshape(ncols, -1)
        vecs[:a.shape[1], VC[name]:VC[name] + ncols] = a.T

    put('g_mix', f('g_mix'), 8)
    put('g_cq', f('g_cq'), 3)
    put('g_ckv', f('g_ckv'), 2)
    perm = np.concatenate([np.arange(64), np.arange(80, 96), np.arange(64, 80)])
    put('g_q', f('g_qnorm'), 1)
    put('g_qp', f('g_qnorm')[perm], 1)
    put('g_k', f('g_knorm'), 1)
    put('g_kp', f('g_knorm')[perm], 1)
    put('g_ao', f('g_attn_out'), 8)
    put('mu', f('rwkv_mu'), 14)
    for n_, src in (('w0', 'w0'), ('a0', 'a0'), ('k_k', 'k_k'), ('k_a', 'k_a'), ('r_k', 'r_k'),
                    ('ln_w', 'ln_x_w'), ('ln_b', 'ln_x_b')):
        put(n_, f(src).reshape(-1), 4)
    inv = (10000.0 ** (-np.arange(16, dtype=np.float64) / 16)).astype(np.float32)
    vecs[64:80, VC['inv']] = inv
    vecs[80:96, VC['inv']] = inv
    vecs[64:80, VC['sgn']] = -1.0
    vecs[80:96, VC['sgn']] = 1.0
    vecs[:, VC['iota16']:VC['iota16'] + 16] = np.arange(16, dtype=np.float32)[None, :]
    w_in = f('w_in')
    w_krp = np.zeros((1024, 96), np.float32)
    w_krp[:, 64:80] = w_in[:, 656:672]
    w_krp[:, 80:96] = w_in[:, 640:656]
    w_uq = f('w_uq')
    cols = np.concatenate([h * 96 + perm for h in range(8)])
    w_uqp = np.ascontiguousarray(w_uq[:, cols])
    keysT = np.ascontiguousarray(f('sub_keys').reshape(16, 128, 128).transpose(0, 2, 1))
    MU = np.zeros((128, 256), np.float32)
    ML = np.zeros((128, 128), np.float32)
    ii = np.arange(64)
    for e in range(2):
        o = e * 64
        MU[o:o + 64, o:o + 64] = (ii[:, None] < ii[None, :])
        MU[o:o + 64, 128 + o:128 + o + 64] = (ii[:, None] <= ii[None, :])
        ML[o:o + 64, o:o + 64] = (ii[:, None] > ii[None, :])
    return dict(MU=MU, ML=ML, vecs=vecs, ident=np.eye(128, dtype=np.float32), w_in=w_in, w_krp=w_krp, w_uq=w_uq, w_uqp=w_uqp,
                w_uk=f('w_uk'), w_uv=f('w_uv'), w2=f('w2'), a2=f('a2'), g2=f('g2'), w_o=f('w_o'),
                g_ffn=f('g_ffn'), w_pq=f('w_pq'), keysT=keysT, expert_uv=np.ascontiguousarray(np.concatenate([f('expert_u'), f('expert_v')], axis=1)))


def prep_core(inp, c, S):
    x = np.asarray(inp['x'], np.float32)[2 * c:2 * c + 2].reshape(2 * S, 1024)
    pos = np.asarray(inp['positions'], np.int32)[2 * c:2 * c + 2].reshape(2 * S)
    return dict(x=np.ascontiguousarray(x), xT=np.ascontiguousarray(x.T), pos=np.ascontiguousarray(pos))


def kernel(**inputs):
    S = inputs['x'].shape[1]
    B = inputs['x'].shape[0]
    ncore = B // 2
    shared = prep_shared(inputs)
    nc = build(S)
    in_maps = []
    for c in range(ncore):
        m = dict(shared)
        m.update(prep_core(inputs, c, S))
        in_maps.append(m)
    res = run_bass_kernel_spmd(nc, in_maps, core_ids=list(range(ncore)))
    out = np.stack([r["out"].reshape(2, S, 1024) for r in res.results], 0).reshape(B, S, 1024)
    return out.astype(np.float32)
```
